# Optimizing a Trainium2 kernel written in Bass

```python
import math
import jax, jax.numpy as jnp
from jax import lax
import numpy as np

D_MODEL = 1024
BATCH = 8
SEQ = 2048
DEPTH = 2

N_HEADS = 16
HEAD_DIM = D_MODEL // N_HEADS
Q_BLOCK = 128
MOBA_BLOCK = 256
MOBA_TOPK = 3
N_BUCKETS = 32
MAX_DISTANCE = 128
D_FF = 2816
N_EXPERTS = 8
TOP_K = 2
D_FF_EXPERT = 3584
N_DENSE = (DEPTH + 1) // 2
N_MOE = DEPTH // 2
RMS_EPS = 1e-6
NEG_BIG = -1e30

kernel_name = 'hybrid_stickbreak_moba_moe_block'


def rmsnorm(x, g):
    xf = x.astype(jnp.float32)
    y = xf * lax.rsqrt(jnp.mean(xf * xf, axis=-1, keepdims=True) + RMS_EPS)
    return (y * g.astype(jnp.float32)).astype(x.dtype)


def t5_bucket(dist):
    n = jnp.maximum(dist, 0)
    max_exact = N_BUCKETS // 2
    nf = jnp.maximum(n, 1).astype(jnp.float32)
    large = max_exact + (jnp.log(nf / max_exact) / math.log(MAX_DISTANCE / max_exact)
                         * (N_BUCKETS - max_exact)).astype(jnp.int32)
    large = jnp.minimum(large, N_BUCKETS - 1)
    return jnp.where(n < max_exact, n, large)


def qkv_heads(h, w_qkv):
    b, s, d = h.shape
    qkv = jnp.einsum('bsd,de->bse', h, w_qkv)
    q, k, v = jnp.split(qkv, 3, axis=-1)
    to_heads = lambda t: t.reshape(b, s, N_HEADS, HEAD_DIM).transpose(0, 2, 1, 3)
    return to_heads(q), to_heads(k), to_heads(v)


def stick_breaking_attention(q, k, v):
    b, nh, s, dh = q.shape
    scale = dh ** -0.5
    k_pos = jnp.arange(s)

    def block(c):
        q0 = c * Q_BLOCK
        qc = lax.dynamic_slice_in_dim(q, q0, Q_BLOCK, axis=2)
        z = jnp.einsum('bhqd,bhkd->bhqk', qc, k).astype(jnp.float32) * scale
        q_pos = q0 + jnp.arange(Q_BLOCK)
        past = k_pos[None, :] < q_pos[:, None]
        log_one_minus = jnp.where(past, jax.nn.log_sigmoid(-z), 0.0)
        after = lax.cumsum(log_one_minus, axis=3, reverse=True) - log_one_minus
        w = jnp.where(past, jnp.exp(jax.nn.log_sigmoid(z) + after), 0.0)
        return jnp.einsum('bhqk,bhkd->bhqd', w.astype(v.dtype), v)

    out = lax.map(block, jnp.arange(s // Q_BLOCK))
    return out.transpose(1, 2, 0, 3, 4).reshape(b, nh, s, dh)


def moba_attention(q, k, v, rel_bias):
    b, nh, s, dh = q.shape
    scale = dh ** -0.5
    n_blk = -(-s // MOBA_BLOCK)
    s_pad = n_blk * MOBA_BLOCK
    pad = ((0, 0), (0, 0), (0, s_pad - s), (0, 0))
    kb = jnp.pad(k, pad).reshape(b, nh, n_blk, MOBA_BLOCK, dh)
    vb = jnp.pad(v, pad).reshape(b, nh, n_blk, MOBA_BLOCK, dh)
    k_mean = jnp.mean(kb, axis=3)
    n_sel = min(MOBA_TOPK, n_blk - 1)
    bias_hb = rel_bias.T
    in_blk = jnp.arange(MOBA_BLOCK)
    bi = jnp.arange(b)[:, None, None, None]
    hi = jnp.arange(nh)[None, :, None, None]

    def block(c):
        q0 = c * Q_BLOCK
        own = q0 // MOBA_BLOCK
        qc = lax.dynamic_slice_in_dim(q, q0, Q_BLOCK, axis=2)
        q_pos = q0 + jnp.arange(Q_BLOCK)
        k_own = lax.dynamic_index_in_dim(kb, own, axis=2, keepdims=False)
        v_own = lax.dynamic_index_in_dim(vb, own, axis=2, keepdims=False)
        dist_own = q_pos[:, None] - (own * MOBA_BLOCK + in_blk)[None, :]
        logit_own = (jnp.einsum('bhqd,bhkd->bhqk', qc, k_own).astype(jnp.float32) * scale
                     + bias_hb[:, t5_bucket(dist_own)])
        logit_own = jnp.where(dist_own >= 0, logit_own, NEG_BIG)
        if n_sel == 0:
            p_own = jax.nn.softmax(logit_own, axis=-1).astype(v.dtype)
            return jnp.einsum('bhqk,bhkd->bhqd', p_own, v_own)
        gate = jnp.einsum('bhqd,bhnd->bhqn', qc, k_mean).astype(jnp.float32)
        gate = jnp.where(jnp.arange(n_blk) < own, gate, NEG_BIG)
        _, idx = lax.top_k(gate, n_sel)
        valid = idx < own
        k_sel = kb[bi, hi, idx]
        v_sel = vb[bi, hi, idx]
        dist_sel = q_pos[:, None, None] - (idx[..., None] * MOBA_BLOCK + in_blk)
        logit_sel = (jnp.einsum('bhqd,bhqnkd->bhqnk', qc, k_sel).astype(jnp.float32) * scale
                     + bias_hb[hi[..., None], t5_bucket(dist_sel)])
        logit_sel = jnp.where(valid[..., None], logit_sel, NEG_BIG)
        n_s = n_sel * MOBA_BLOCK
        logits = jnp.concatenate([logit_sel.reshape(b, nh, Q_BLOCK, n_s), logit_own], axis=-1)
        p = jax.nn.softmax(logits, axis=-1).astype(v.dtype)
        p_sel = p[..., :n_s].reshape(b, nh, Q_BLOCK, n_sel, MOBA_BLOCK)
        p_own = p[..., n_s:]
        return (jnp.einsum('bhqnk,bhqnkd->bhqd', p_sel, v_sel)
                + jnp.einsum('bhqk,bhkd->bhqd', p_own, v_own))

    out = lax.map(block, jnp.arange(s // Q_BLOCK))
    return out.transpose(1, 2, 0, 3, 4).reshape(b, nh, s, dh)


def swiglu(h, w1, w3, w2):
    hid = jax.nn.silu(jnp.einsum('bsd,df->bsf', h, w1)) * jnp.einsum('bsd,df->bsf', h, w3)
    return jnp.einsum('bsf,fd->bsd', hid, w2)


def moe_swiglu(h, w_router, w1, w3, w2):
    b, s, d = h.shape
    t = h.reshape(b * s, d)
    logits = jnp.dot(t, w_router).astype(jnp.float32)
    top_val, top_idx = lax.top_k(logits, TOP_K)
    top_w = jax.nn.softmax(top_val, axis=-1)
    gates = jnp.einsum('tk,tke->te', top_w,
                       jax.nn.one_hot(top_idx, N_EXPERTS, dtype=jnp.float32)).astype(h.dtype)

    def expert(args):
        e_w1, e_w3, e_w2, g = args
        hid = jax.nn.silu(t @ e_w1) * (t @ e_w3)
        return (hid @ e_w2) * g[:, None]

    y = jnp.sum(lax.map(expert, (w1, w3, w2, gates.T)), axis=0)
    return y.reshape(b, s, d)


def setup_inputs(seed: int = 0) -> dict:
    key = jax.random.key(seed)
    ks = jax.random.split(key, 14)
    nrm = lambda k, shape, fan_in: jax.random.normal(k, shape, jnp.float32) * fan_in ** -0.5
    x = jax.random.normal(ks[0], (BATCH, SEQ, D_MODEL), jnp.float32)
    w_qkv = nrm(ks[1], (DEPTH, D_MODEL, 3 * D_MODEL), D_MODEL)
    w_o = nrm(ks[2], (DEPTH, D_MODEL, D_MODEL), D_MODEL)
    mixer_norm = 1.0 + 0.02 * jax.random.normal(ks[3], (DEPTH, D_MODEL), jnp.float32)
    ffn_norm = 1.0 + 0.02 * jax.random.normal(ks[4], (DEPTH, D_MODEL), jnp.float32)
    rel_bias = 0.5 * jax.random.normal(ks[5], (N_BUCKETS, N_HEADS), jnp.float32)
    w1 = nrm(ks[6], (N_DENSE, D_MODEL, D_FF), D_MODEL)
    w3 = nrm(ks[7], (N_DENSE, D_MODEL, D_FF), D_MODEL)
    w2 = nrm(ks[8], (N_DENSE, D_FF, D_MODEL), D_FF)
    router = nrm(ks[9], (N_MOE, D_MODEL, N_EXPERTS), D_MODEL)
    e_w1 = nrm(ks[10], (N_MOE, N_EXPERTS, D_MODEL, D_FF_EXPERT), D_MODEL)
    e_w3 = nrm(ks[11], (N_MOE, N_EXPERTS, D_MODEL, D_FF_EXPERT), D_MODEL)
    e_w2 = nrm(ks[12], (N_MOE, N_EXPERTS, D_FF_EXPERT, D_MODEL), D_FF_EXPERT)
    final_norm = 1.0 + 0.02 * jax.random.normal(ks[13], (D_MODEL,), jnp.float32)
    return {'x': x, 'w_qkv': w_qkv, 'w_o': w_o, 'mixer_norm': mixer_norm,
            'ffn_norm': ffn_norm, 'rel_bias': rel_bias, 'w1': w1, 'w3': w3, 'w2': w2,
            'router': router, 'e_w1': e_w1, 'e_w3': e_w3, 'e_w2': e_w2,
            'final_norm': final_norm}


def reference(x, w_qkv, w_o, mixer_norm, ffn_norm, rel_bias, w1, w3, w2,
              router, e_w1, e_w3, e_w2, final_norm):
    b, s, d = x.shape
    h = x
    for i in range(DEPTH):
        hn = rmsnorm(h, mixer_norm[i])
        q, k, v = qkv_heads(hn, w_qkv[i])
        if i % 2 == 0:
            o = stick_breaking_attention(q, k, v)
        else:
            o = moba_attention(q, k, v, rel_bias)
        o = o.transpose(0, 2, 1, 3).reshape(b, s, d)
        h = h + jnp.einsum('bsd,de->bse', o, w_o[i])
        hn = rmsnorm(h, ffn_norm[i])
        j = i // 2
        if i % 2 == 0:
            h = h + swiglu(hn, w1[j], w3[j], w2[j])
        else:
            h = h + moe_swiglu(hn, router[j], e_w1[j], e_w3[j], e_w2[j])
    return rmsnorm(h, final_norm)
```

```python
import numpy as np
import concourse.bass as bass
import concourse.mybir as mybir
from concourse.bass_utils import run_bass_kernel_spmd

F32 = mybir.dt.float32
BF16 = mybir.dt.bfloat16
AF = mybir.ActivationFunctionType
ALU = mybir.AluOpType

ENGS = ("pe", "dve", "act", "pool", "sp")
N_DMA_SEMS = 6


class Op:
    __slots__ = ("eng", "fn", "deps", "dma", "sig", "sem", "val", "idx", "prewait")

    def __init__(self, eng, fn, dma):
        self.eng = eng
        self.fn = fn
        self.dma = dma
        self.deps = []
        self.sig = False
        self.sem = None
        self.val = 0
        self.prewait = None


class Rec:
    def __init__(self, nc, same_engine_sync=True):
        self.nc = nc
        self.ops = []
        self.last_w = {}
        self.readers = {}
        self.same_engine_sync = same_engine_sync

    def add(self, eng, fn, r=(), w=(), dma=False):
        op = Op(eng, fn, dma)
        op.idx = len(self.ops)
        deps = set()
        for b in r:
            if b in self.last_w:
                deps.add(self.last_w[b])
        for b in w:
            if b in self.last_w:
                deps.add(self.last_w[b])
            for x in self.readers.get(b, ()):
                deps.add(x)
        deps.discard(op.idx)
        op.deps = sorted(deps)
        for b in r:
            self.readers.setdefault(b, []).append(op.idx)
        for b in w:
            self.last_w[b] = op.idx
            self.readers[b] = []
        self.ops.append(op)
        return op

    def pe(self, fn, r=(), w=()):
        return self.add("pe", fn, r, w)

    def dve(self, fn, r=(), w=()):
        return self.add("dve", fn, r, w)

    def act(self, fn, r=(), w=()):
        return self.add("act", fn, r, w)

    def pool(self, fn, r=(), w=()):
        return self.add("pool", fn, r, w)

    def dma(self, q, fn, r=(), w=()):
        return self.add(q, fn, r, w, dma=True)

    def _needs_wait(self, op, d):
        if d.dma:
            return True
        if d.eng != op.eng:
            return True
        if op.dma:
            return True
        if op.eng == "pe":
            return False
        return self.same_engine_sync

    def emit(self, sems, dma_sems, cnt=None, dcnt=None):
        nc = self.nc
        ops = self.ops
        for op in ops:
            for di in op.deps:
                d = ops[di]
                if self._needs_wait(op, d):
                    d.sig = True
        if cnt is None:
            cnt = {e: 0 for e in ENGS}
        if dcnt is None:
            dcnt = {e: 0 for e in ENGS}
        for op in ops:
            if op.dma:
                i = dcnt[op.eng]
                dcnt[op.eng] += 1
                k = len(dma_sems[op.eng])
                op.sem = dma_sems[op.eng][i % k]
                op.val = 16 * (i // k + 1)
                if i >= k:
                    op.prewait = (op.sem, 16 * (i // k))
            elif op.sig:
                cnt[op.eng] += 1
                op.sem = sems[op.eng]
                op.val = cnt[op.eng]
        per_eng = {e: [] for e in ENGS}
        for op in ops:
            per_eng[op.eng].append(op)
        return per_eng

    def run_engine(self, eng_name, eng, per_eng):
        ops = self.ops
        waited = {}
        for op in per_eng[eng_name]:
            waits = {}
            if op.prewait is not None:
                s, v = op.prewait
                waits[id(s)] = (s, v)
            for di in op.deps:
                d = ops[di]
                if not self._needs_wait(op, d):
                    continue
                key = id(d.sem)
                if key not in waits or waits[key][1] < d.val:
                    waits[key] = (d.sem, d.val)
            for key, (s, v) in waits.items():
                if waited.get(key, 0) >= v:
                    continue
                eng.wait_ge(s, v)
                waited[key] = v
            ins = op.fn(eng)
            if op.dma:
                ins.then_inc(op.sem, 16)
            elif op.sig:
                ins.then_inc(op.sem, 1)


def run_phase(nc, build, same_engine_sync=True, final_waits=True):
    rec = Rec(nc, same_engine_sync)
    from contextlib import ExitStack

    with ExitStack() as es:
        sems = {e: es.enter_context(nc.semaphore(f"s_{e}_{id(rec) % 100000}")) for e in ENGS}
        dma_sems = {
            e: [es.enter_context(nc.semaphore(f"d_{e}{i}_{id(rec) % 100000}")) for i in range(N_DMA_SEMS)]
            for e in ("sp", "pool", "act")
        }
        dma_sems["pe"] = []
        dma_sems["dve"] = []
        build(rec)
        per_eng = rec.emit(sems, dma_sems)
        final = {}
        for op in rec.ops:
            if op.dma:
                final[(op.eng, id(op.sem))] = (op.sem, op.val)
        with nc.Block() as block:

            def mk(eng_name):
                def body(eng):
                    rec.run_engine(eng_name, eng, per_eng)
                    for (q, _), (s, v) in final.items():
                        if q == eng_name:
                            eng.wait_ge(s, v)

                return body

            block.tensor(mk("pe"))
            block.vector(mk("dve"))
            block.scalar(mk("act"))
            block.gpsimd(mk("pool"))
            block.sync(mk("sp"))
    return rec


S = 2048
D = 1024
NH = 16
DH = 64
DC = 8
NTT = 4
TT = 512
DFF = 2816
DFE = 3584
NE = 8
EPS = 1e-6
NEG = -30000.0
FILL_SB = 1
FILL_MOBA = 0
FILL_MOE = 0
NORM_FILL = 0
GATE_JUNK = 2
FILL_DELAY = 10
MOE_CAP = 640
N_BUCKETS = 32
MAX_DISTANCE = 128


def _t5_bucket_np(dist):
    import math
    n = np.maximum(dist, 0)
    max_exact = N_BUCKETS // 2
    nf = np.maximum(n, 1).astype(np.float32)
    large = max_exact + (np.log(nf / np.float32(max_exact)) / np.float32(math.log(MAX_DISTANCE / max_exact))
                         * np.float32(N_BUCKETS - max_exact)).astype(np.int32)
    large = np.minimum(large, N_BUCKETS - 1)
    return np.where(n < max_exact, n, large)


class Ctx:
    pass


def build_nc(stop_after=None, dbg=False):
    from contextlib import ExitStack

    nc = bass.Bass("TRN2", target_bir_lowering=False)
    g = Ctx()
    g.nc = nc
    dram = lambda name, shape, kind="ExternalInput", dt=F32: nc.dram_tensor(name, shape, dt, kind=kind).ap()
    g.x = dram("x", [S, D])
    g.w_qkv = dram("w_qkv", [2, D, 3 * D])
    g.w_o = dram("w_o", [2, D, D])
    g.gains = dram("gains", [128, 5 * DC])
    g.biasT = dram("biasT", [NH, 128, 2, 128])
    g.b31 = dram("b31", [128, NH])
    g.w1 = dram("w1", [D, DFF])
    g.w3 = dram("w3", [D, DFF])
    g.w2 = dram("w2", [DFF, D])
    g.router = dram("router", [D, NE])
    g.e_w1 = dram("e_w1", [NE, D, DFE])
    g.e_w3 = dram("e_w3", [NE, D, DFE])
    g.e_w2 = dram("e_w2", [NE, DFE, D])
    g.y = dram("y", [S, D], kind="ExternalOutput")
    if dbg:
        g.dbg = dram("dbg", [128, DC, S], kind="ExternalOutput")

    with ExitStack() as es:
        T = lambda name, shape, dt: es.enter_context(nc.sbuf_tensor(name, shape, dt))
        g.hT = T("hT", [128, DC, S], F32)
        g.hnT = T("hnT", [128, DC, S], BF16)
        g.oT = T("oT", [128, DC, S], BF16)
        g.gains_sb = T("gains_sb", [128, 5 * DC], F32)
        g.ident_f = T("ident_f", [128, 128], F32)
        g.ident_b = T("ident_b", [128, 128], BF16)
        g.ones_f = T("ones_f", [128, 128], F32)
        g.ones_b = T("ones_b", [128, 128], BF16)
        g.negones_b = T("negones_b", [128, 128], BF16)
        g.zeros_b = T("zeros_b", [128, 128], BF16)
        g.neguincl = T("neguincl", [128, 128], BF16)
        g.mstrict = T("mstrict", [128, 128], BF16)
        g.gt = T("gt_g", [128, 16, NE], F32)
        g.gtb = T("gtb_g", [128, 16, NE], BF16)
        g.rankm = T("rankm_g", [128, 16, NE], F32)
        g.flag_i = T("flag_i", [128, 2], mybir.dt.int32)
        g.ps = [es.enter_context(nc.psum_tensor(f"ps{i}", [128, 512], F32)) for i in range(8)]
        g.bar = es.enter_context(nc.semaphore("bar"))
        g.nphase = 0
        g.sems = {e: es.enter_context(nc.semaphore(f"s_{e}")) for e in ENGS}
        g.dma_sems = {e: [es.enter_context(nc.semaphore(f"d_{e}{i}")) for i in range(N_DMA_SEMS)]
                      for e in ("sp", "pool", "act")}
        g.dma_sems["pe"] = []
        g.dma_sems["dve"] = []
        g.sems2 = {e: es.enter_context(nc.semaphore(f"t_{e}")) for e in ENGS}
        g.dma_sems2 = {e: [es.enter_context(nc.semaphore(f"u_{e}{i}")) for i in range(N_DMA_SEMS)]
                       for e in ("sp", "pool", "act")}
        g.dma_sems2["pe"] = []
        g.dma_sems2["dve"] = []
        g.cnt = {e: 0 for e in ENGS}
        g.dcnt = {e: 0 for e in ENGS}

        phases = [
            ("init", lambda g: phase_init(g)),
            ("attn0", lambda g: phase_attn(g, 0)),
            ("ffn0", lambda g: phase_ffn_dense(g)),
            ("attn1", lambda g: phase_attn(g, 1)),
            ("moe", lambda g: phase_moe(g)),
            ("final", lambda g: phase_final(g)),
        ]
        g.stop = stop_after
        for name, fn in phases:
            fn(g)
            if stop_after == name or (stop_after or "").startswith(name + "_"):
                break
        if dbg:
            def b(rec):
                rec.dma("sp", lambda e: e.dma_start(out=g.dbg, in_=g.hT[:]), r=["hT_all"])
            phase(g, b)
    return nc


def phase(g, build):
    nc = g.nc
    k = g.nphase
    g.nphase += 1
    rec = Rec(nc, True)
    from contextlib import ExitStack

    if True:
        sems, dma_sems = g.sems, g.dma_sems
        build(rec)
        per_eng = rec.emit(sems, dma_sems, g.cnt, g.dcnt)
        final = {}
        for op in rec.ops:
            if op.dma:
                final[(op.eng, id(op.sem))] = (op.sem, op.val)
        with nc.Block() as block:
            def mk(eng_name):
                def body(eng):
                    if k > 0:
                        eng.wait_ge(g.bar, 5 * k)
                    rec.run_engine(eng_name, eng, per_eng)
                    for (q, _), (s, v) in final.items():
                        if q == eng_name:
                            eng.wait_ge(s, v)
                    eng.drain().then_inc(g.bar, 1)
                return body
            block.tensor(mk("pe"))
            block.vector(mk("dve"))
            block.scalar(mk("act"))
            block.gpsimd(mk("pool"))
            block.sync(mk("sp"))
    return rec


class Rot:
    def __init__(self, n):
        self.n = n
        self.i = -1

    def next(self):
        self.i = (self.i + 1) % self.n
        return self.i


def emit_rmsnorm(g, rec, norm_idx, sq, lnv, rstd, psbanks, out_bf=True, out_f32=None):
    def sq_part(tt):
        cs = slice(tt * TT, (tt + 1) * TT)
        bank = psbanks[tt % len(psbanks)]
        bname = ("psn", tt % len(psbanks))

        def sqmm(c):
            s = sq[c % len(sq)]
            sn = ("sq", c % len(sq))
            rec.act(lambda e: e.activation(out=s[:], in_=g.hT[:, c, cs], func=AF.Square),
                    r=[("hT", c, tt)], w=[sn])
            rec.pe(lambda e: e.matmul(bank[:], lhsT=g.ones_f[:], rhs=s[:], start=(c == 0), stop=(c == DC - 1)),
                   r=[sn, "consts"], w=[bname])
            for _ in range(NORM_FILL):
                rec.pe(lambda e: e.matmul(g.ps[7][:], lhsT=g.ident_b[:], rhs=g.oT[:, 0, 0:TT], start=True, stop=True),
                       r=["consts"], w=["ps_junk"])
        for c in range(DC):
            sqmm(c)

    def out_part(tt):
        cs = slice(tt * TT, (tt + 1) * TT)
        bank = psbanks[tt % len(psbanks)]
        bname = ("psn", tt % len(psbanks))
        l = lnv[tt % len(lnv)]
        r_ = rstd[tt % len(rstd)]
        ln_n = ("lnv", tt % len(lnv))
        rs_n = ("rstd", tt % len(rstd))
        rec.act(lambda e: e.activation(out=l[:], in_=bank[:], func=AF.Ln, scale=1.0 / D, bias=EPS),
                r=[bname, "consts"], w=[ln_n])
        rec.act(lambda e: e.activation(out=r_[:], in_=l[:], func=AF.Exp, scale=-0.5),
                r=[ln_n], w=[rs_n])

        def outc(c):
            gcol = g.gains_sb[:, norm_idx * DC + c: norm_idx * DC + c + 1]
            if out_bf:
                rec.dve(lambda e: e.scalar_tensor_tensor(
                    out=g.hnT[:, c, cs], in0=g.hT[:, c, cs], scalar=gcol, in1=r_[:], op0=ALU.mult, op1=ALU.mult),
                    r=[("hT", c, tt), rs_n, "consts"], w=[("hnT", c, tt)])
            if out_f32 is not None:
                out_f32(rec, tt, c, gcol, r_, rs_n)
        for c in range(DC):
            outc(c)
    for tt in range(NTT + 1):
        if tt < NTT:
            sq_part(tt)
        if tt >= 1:
            out_part(tt - 1)


def phase_init(g):
    nc = g.nc
    from contextlib import ExitStack
    with ExitStack() as es:
        T = lambda name, shape, dt: es.enter_context(nc.sbuf_tensor(f"{name}_p{g.nphase}", shape, dt))
        stage = [T(f"xstage{i}", [128, 4, D], F32) for i in range(2)]

        def b(rec):
            rec.dma("sp", lambda e: e.dma_start(out=g.gains_sb[:], in_=g.gains), w=["consts_g"])
            rec.pool(lambda e: e.memset(g.ident_f[:], 0.0), w=["ident_f"])
            rec.pool(lambda e: e.affine_select(out=g.ident_f[:], in_=g.ident_f[:], pattern=[[1, 128]],
                                               compare_op=ALU.not_equal, fill=1.0, base=0, channel_multiplier=-1),
                     r=["ident_f"], w=["ident_f"])
            rec.pool(lambda e: e.tensor_copy(out=g.ident_b[:], in_=g.ident_f[:]), r=["ident_f"], w=["c1"])
            rec.pool(lambda e: e.memset(g.ones_f[:], 1.0), w=["c2"])
            rec.pool(lambda e: e.memset(g.ones_b[:], 1.0), w=["c3"])
            rec.pool(lambda e: e.memset(g.negones_b[:], -1.0), w=["c4"])
            rec.pool(lambda e: e.memset(g.zeros_b[:], 0.0), w=["c5"])
            rec.pool(lambda e: e.memset(g.neguincl[:], -1.0), w=["c6"])
            rec.pool(lambda e: e.affine_select(out=g.neguincl[:], in_=g.neguincl[:], pattern=[[-1, 128]],
                                               compare_op=ALU.is_ge, fill=0.0, base=0, channel_multiplier=1),
                     r=["c6"], w=["c6"])
            rec.pool(lambda e: e.memset(g.mstrict[:], 1.0), w=["c7"])
            rec.pool(lambda e: e.affine_select(out=g.mstrict[:], in_=g.mstrict[:], pattern=[[1, 128]],
                                               compare_op=ALU.is_gt, fill=0.0, base=0, channel_multiplier=-1),
                     r=["c7"], w=["c7"])
            xv = g.x.rearrange("(t j p) d -> t p j d", j=4, p=128)
            k = 0
            for tt in range(NTT):
                st = stage[tt % 2]
                sn = ("stage", tt % 2)
                rec.dma("sp", lambda e, st=st, tt=tt: e.dma_start(out=st[:], in_=xv[tt]), w=[sn])
                for c in range(DC):
                    bank = g.ps[k % 4]
                    bn = ("ps", k % 4)
                    for j in range(4):
                        rec.pe(lambda e, st=st, j=j, c=c, bank=bank: e.transpose(
                            bank[:, j * 128:(j + 1) * 128], st[:, j, c * 128:(c + 1) * 128], g.ident_f[:]),
                            r=[sn, "ident_f"], w=[bn])
                    dst = g.hT[:, c, tt * TT:(tt + 1) * TT]
                    if k % 2 == 0:
                        rec.act(lambda e, dst=dst, bank=bank: e.activation(out=dst, in_=bank[:], func=AF.Copy),
                                r=[bn], w=[("hT", c, tt)])
                    else:
                        rec.dve(lambda e, dst=dst, bank=bank: e.tensor_copy(out=dst, in_=bank[:]),
                                r=[bn], w=[("hT", c, tt)])
                    k += 1
        phase(g, b)


def make_in_map(inp, b):
    norms = np.stack([inp["mixer_norm"][0], inp["ffn_norm"][0], inp["mixer_norm"][1],
                      inp["ffn_norm"][1], inp["final_norm"]], axis=0)
    gains = np.ascontiguousarray(norms.reshape(5, DC, 128).transpose(2, 0, 1).reshape(128, 5 * DC))
    kk = np.arange(128)[:, None, None]
    jj = np.arange(2)[None, :, None]
    qq = np.arange(128)[None, None, :]
    dist = qq - kk + 128 * jj
    bt = inp["rel_bias"][_t5_bucket_np(dist), :]
    bt = np.where((dist >= 0)[..., None], bt, np.float32(-1e30)).astype(np.float32)
    biasT = np.ascontiguousarray(bt.transpose(3, 0, 1, 2))
    b31 = np.ascontiguousarray(np.broadcast_to(inp["rel_bias"][N_BUCKETS - 1][None, :], (128, NH))).astype(np.float32)
    return {
        "x": np.ascontiguousarray(inp["x"][b]),
        "w_qkv": inp["w_qkv"], "w_o": inp["w_o"], "gains": gains, "biasT": biasT, "b31": b31,
        "w1": inp["w1"][0], "w3": inp["w3"][0], "w2": inp["w2"][0], "router": inp["router"][0],
        "e_w1": inp["e_w1"][0], "e_w3": inp["e_w3"][0], "e_w2": inp["e_w2"][0],
    }


def phase_norm(g, norm_idx, router=False):
    nc = g.nc
    from contextlib import ExitStack
    with ExitStack() as es:
        T = lambda name, shape, dt: es.enter_context(nc.sbuf_tensor(f"{name}_p{g.nphase}", shape, dt))
        sq = [T(f"sq{i}", [128, TT], F32) for i in range(3)]
        lnv = [T(f"lnv{i}", [128, TT], F32) for i in range(2)]
        rstd = [T(f"rstd{i}", [128, TT], F32) for i in range(2)]
        if router:
            hn32 = T("hn32", [128, DC, TT], F32)
            wr = T("wr", [128, DC, NE], F32)
            lg = T("lg", [128, 16, NE], F32)
            m8 = T("m8", [128, 16, 8], F32)
            sm = T("sm", [128, 16, 4], F32)
            t1 = T("t1", [128, NE], F32)
            t2 = T("t2", [128, NE], F32)
            gt = g.gt
            mf = T("mf", [128, 16, NE], F32)
            mb = T("mb", [128, 16, NE], BF16)
            macc = T("macc", [128, 17, NE], F32)
            maccb = T("maccb", [128, 17, NE], BF16)
            rtmp = T("rtmp", [128, 16, NE], F32)
            cmax = T("cmax", [128, 2], F32)

        def b(rec):
            out_f32 = None
            if router:
                rec.dma("sp", lambda e: e.dma_start(out=wr[:], in_=g.router.rearrange("(c p) e -> p c e", p=128)),
                        w=["wr"])

                def out_f32(rec, tt, c, gcol, r_, rs_n):
                    cs = slice(tt * TT, (tt + 1) * TT)
                    rec.dve(lambda e: e.scalar_tensor_tensor(
                        out=hn32[:, c, :], in0=g.hT[:, c, cs], scalar=gcol, in1=r_[:], op0=ALU.mult, op1=ALU.mult),
                        r=[("hT", c, tt), rs_n, "consts"], w=[("hn32", c)])
                    if c == DC - 1:
                        bank = g.ps[4 + tt % 2]
                        bn = ("psr", tt % 2)
                        for j in range(4):
                            for cc in range(DC):
                                rec.pe(lambda e, j=j, cc=cc: e.matmul(
                                    bank[:, j * 8:(j + 1) * 8], lhsT=hn32[:, cc, j * 128:(j + 1) * 128], rhs=wr[:, cc, :],
                                    start=(cc == 0), stop=(cc == DC - 1)),
                                    r=[("hn32", cc), "wr"], w=[bn])
                        rec.dve(lambda e: e.tensor_copy(out=lg[:, tt * 4:(tt + 1) * 4, :],
                                                        in_=bank[:, 0:32].rearrange("p (j e) -> p j e", j=4)),
                                r=[bn], w=[("lg", tt)])

            emit_rmsnorm(g, rec, norm_idx, sq, lnv, rstd, [g.ps[0], g.ps[1]], out_bf=True, out_f32=out_f32)
            if router:
                G = g.oT
                for tk in range(16):
                    tt = tk // 4
                    rec.dve(lambda e, tk=tk: e.max(out=m8[:, tk, :], in_=lg[:, tk, :]), r=[("lg", tt)], w=[("m8", tk)])
                    rec.dve(lambda e, tk=tk: e.tensor_tensor(out=sm[:, tk, 0:1], in0=m8[:, tk, 0:1], in1=m8[:, tk, 1:2],
                                                             op=ALU.subtract), r=[("m8", tk)], w=[("sm0", tk)])
                    rec.act(lambda e, tk=tk: e.activation(out=sm[:, tk, 1:2], in_=sm[:, tk, 0:1], func=AF.Exp, scale=-1.0),
                            r=[("sm0", tk)], w=[("sm1", tk)])
                    rec.dve(lambda e, tk=tk: e.tensor_scalar(out=sm[:, tk, 2:3], in0=sm[:, tk, 1:2], scalar1=1.0, scalar2=None,
                                                             op0=ALU.add), r=[("sm1", tk)], w=[("sm2", tk)])
                    rec.dve(lambda e, tk=tk: e.reciprocal(out=sm[:, tk, 2:3], in_=sm[:, tk, 2:3]),
                            r=[("sm2", tk)], w=[("sm2", tk)])
                    rec.dve(lambda e, tk=tk: e.tensor_tensor(out=sm[:, tk, 3:4], in0=sm[:, tk, 1:2], in1=sm[:, tk, 2:3],
                                                             op=ALU.mult), r=[("sm1", tk), ("sm2", tk)], w=[("sm3", tk)])
                    rec.dve(lambda e, tk=tk: e.tensor_scalar(out=t1[:], in0=lg[:, tk, :], scalar1=m8[:, tk, 0:1],
                                                             scalar2=sm[:, tk, 2:3], op0=ALU.is_equal, op1=ALU.mult),
                            r=[("lg", tt), ("m8", tk), ("sm2", tk)], w=["t1"])
                    rec.dve(lambda e, tk=tk: e.tensor_scalar(out=t2[:], in0=lg[:, tk, :], scalar1=m8[:, tk, 1:2],
                                                             scalar2=sm[:, tk, 3:4], op0=ALU.is_equal, op1=ALU.mult),
                            r=[("lg", tt), ("m8", tk), ("sm3", tk)], w=["t2"])
                    rec.dve(lambda e, tk=tk: e.tensor_tensor(out=gt[:, tk, :], in0=t1[:], in1=t2[:], op=ALU.add),
                            r=["t1", "t2"], w=[("gt", tk)])
                rec.dve(lambda e: e.tensor_copy(out=g.gtb[:], in_=gt[:]), r=[("gt", tk) for tk in range(16)], w=["gtb"])
                rec.dve(lambda e: e.tensor_scalar(out=mf[:], in0=gt[:], scalar1=0.0, scalar2=None, op0=ALU.is_gt),
                        r=[("gt", tk) for tk in range(16)], w=["mf"])
                rec.dve(lambda e: e.tensor_copy(out=mb[:], in_=mf[:]), r=["mf"], w=["mb"])
                rec.dve(lambda e: e.memset(macc[:, 0, :], 0.0), w=["macc"])
                for tk in range(16):
                    def acc(tk=tk):
                        rec.dve(lambda e: e.tensor_tensor(out=macc[:, tk + 1, :], in0=macc[:, tk, :], in1=mf[:, tk, :], op=ALU.add),
                                r=["mf", "macc"], w=["macc"])
                    acc()
                rec.dve(lambda e: e.tensor_copy(out=maccb[:], in_=macc[:]), r=["macc"], w=["maccb"])
                rbank = g.ps[2]
                for tk in range(16):
                    def rk(tk=tk):
                        rec.pe(lambda e: e.matmul(rbank[:, tk * 8:(tk + 1) * 8], lhsT=g.mstrict[:], rhs=mb[:, tk, :], start=True, stop=False),
                               r=["mb", "consts"], w=["rbank"])
                        rec.pe(lambda e: e.matmul(rbank[:, tk * 8:(tk + 1) * 8], lhsT=g.ones_b[:], rhs=maccb[:, tk, :], start=False, stop=True),
                               r=["maccb", "consts"], w=["rbank"])
                    rk()
                rec.dve(lambda e: e.scalar_tensor_tensor(out=rtmp[:], in0=rbank[:, 0:128].rearrange("p (t e) -> p t e", e=NE),
                                                         scalar=1.0, in1=mf[:], op0=ALU.add, op1=ALU.mult),
                        r=["rbank", "mf"], w=["rtmp"])
                rec.dve(lambda e: e.tensor_scalar(out=g.rankm[:], in0=rtmp[:], scalar1=-1.0, scalar2=None, op0=ALU.add),
                        r=["rtmp"], w=["rankm"])
                cbank = g.ps[3]
                rec.pe(lambda e: e.matmul(cbank[:, 0:8], lhsT=g.ones_b[:], rhs=maccb[:, 16, :], start=True, stop=True),
                       r=["maccb", "consts"], w=["cbank"])
                rec.dve(lambda e: e.tensor_reduce(out=cmax[:, 0:1], in_=cbank[:, 0:8], axis=mybir.AxisListType.X, op=ALU.max),
                        r=["cbank"], w=["cmax"])
                rec.dve(lambda e: e.tensor_scalar(out=cmax[:, 1:2], in0=cmax[:, 0:1], scalar1=float(MOE_CAP), scalar2=None,
                                                  op0=ALU.is_gt), r=["cmax"], w=["cmax"])
                rec.dve(lambda e: e.tensor_copy(out=g.flag_i[:, 0:1], in_=cmax[:, 1:2]), r=["cmax"], w=["flag"])
        phase(g, b)


def phase_wo(g, layer):
    nc = g.nc
    from contextlib import ExitStack
    with ExitStack() as es:
        wo = es.enter_context(nc.sbuf_tensor(f"wo_p{g.nphase}", [128, DC, D], BF16))

        def b(rec):
            src = g.w_o[layer].rearrange("(c p) e -> p c e", p=128)
            for h2 in range(2):
                rec.dma("pool", lambda e, h2=h2: e.dma_start(out=wo[:, h2 * 4:(h2 + 1) * 4, :], in_=src[:, h2 * 4:(h2 + 1) * 4, :]),
                        w=[("wo", h2)])
            k = 0
            for tt in range(NTT):
                cs = slice(tt * TT, (tt + 1) * TT)
                for ec in range(DC):
                    bank = g.ps[k % 4]
                    bn = ("ps", k % 4)
                    for c in range(DC):
                        rec.pe(lambda e, c=c, ec=ec, bank=bank, cs=cs: e.matmul(
                            bank[:], lhsT=wo[:, c, ec * 128:(ec + 1) * 128], rhs=g.oT[:, c, cs],
                            start=(c == 0), stop=(c == DC - 1)), r=[("wo", c // 4)], w=[bn])
                    rec.dve(lambda e, ec=ec, bank=bank, cs=cs: e.tensor_tensor(
                        out=g.hT[:, ec, cs], in0=bank[:], in1=g.hT[:, ec, cs], op=ALU.add), r=[bn], w=[("hT", ec, tt)])
                    k += 1
        phase(g, b)


class FfnBufs:
    pass


def alloc_ffn_bufs(g, es, NS=3):
    nc = g.nc
    T = lambda name, shape, dt: es.enter_context(nc.sbuf_tensor(f"{name}_p{g.nphase}", shape, dt))
    B = FfnBufs()
    B.NS = NS
    B.w1g = [T(f"w1g{i}", [128, DC, 256], BF16) for i in range(NS)]
    B.w3g = [T(f"w3g{i}", [128, DC, 256], BF16) for i in range(NS)]
    B.w2g = [T(f"w2g{i}", [128, 2, D], BF16) for i in range(NS)]
    B.hid = [T(f"hid{i}", [128, 2, TT], BF16) for i in range(2)]
    B.sl = [T(f"sl{i}", [128, TT], F32) for i in range(2)]
    B.slg = [T(f"slg{i}", [128, TT], F32) for i in range(2)]
    return B


def emit_w_load(g, rec, B, grp, gidx):
    w1, w3, w2, fg, gi = grp
    s = gidx % B.NS
    fs = slice(fg * 256, (fg + 1) * 256)
    rec.dma("pool", lambda e: e.dma_start(out=B.w1g[s][:], in_=w1.rearrange("(c p) f -> p c f", p=128)[:, :, fs]),
            w=[("w1g", s)])
    rec.dma("pool", lambda e: e.dma_start(out=B.w3g[s][:], in_=w3.rearrange("(c p) f -> p c f", p=128)[:, :, fs]),
            w=[("w3g", s)])
    rec.dma("pool", lambda e: e.dma_start(out=B.w2g[s][:], in_=w2[fs, :].rearrange("(i p) e -> p i e", p=128)),
            w=[("w2g", s)])


def emit_ffn(g, rec, B, experts):
    NS = B.NS
    w1g, w3g, w2g, hid, sl, slg = B.w1g, B.w3g, B.w2g, B.hid, B.sl, B.slg
    psA = [g.ps[0], g.ps[1]]
    psB = [g.ps[2], g.ps[3]]
    psO = [g.ps[4], g.ps[5], g.ps[6], g.ps[7]]
    groups = []
    for (w1, w3, w2, F, gi) in experts:
        for fg in range(F // 256):
            groups.append((w1, w3, w2, fg, gi))

    def load(gidx):
        emit_w_load(g, rec, B, groups[gidx], gidx)

    steps = [(gi_, tt) for gi_ in range(len(groups)) for tt in range(NTT)]
    cnt = {"a": 0, "o": 0, "sl": 0}

    def stage_ab(n):
        gidx, tt = steps[n]
        s = gidx % NS
        gate = groups[gidx][4]
        cs = slice(tt * TT, (tt + 1) * TT)
        hs = n % 2
        for fi in range(2):
            ai = cnt["a"] % 2
            cnt["a"] += 1
            ba, bb = psA[ai], psB[ai]
            for c in range(DC):
                rec.pe(lambda e, c=c, fi=fi, ba=ba: e.matmul(ba[:], lhsT=w1g[s][:, c, fi * 128:(fi + 1) * 128],
                                                             rhs=g.hnT[:, c, cs], start=(c == 0), stop=(c == DC - 1)),
                       r=[("w1g", s)], w=[("psA", ai)])
            for c in range(DC):
                rec.pe(lambda e, c=c, fi=fi, bb=bb: e.matmul(bb[:], lhsT=w3g[s][:, c, fi * 128:(fi + 1) * 128],
                                                             rhs=g.hnT[:, c, cs], start=(c == 0), stop=(c == DC - 1)),
                       r=[("w3g", s)], w=[("psB", ai)])
            si = cnt["sl"] % 2
            cnt["sl"] += 1
            rec.act(lambda e, si=si, ba=ba: e.activation(out=sl[si][:], in_=ba[:], func=AF.Silu),
                    r=[("psA", ai)], w=[("sl", si)])
            if gate is None:
                rec.dve(lambda e, si=si, bb=bb, fi=fi: e.tensor_tensor(out=hid[hs][:, fi, :], in0=bb[:], in1=sl[si][:],
                                                                       op=ALU.mult),
                        r=[("psB", ai), ("sl", si)], w=[("hid", hs, fi)])
            else:
                rec.pool(lambda e, si=si: e.tensor_tensor(out=slg[si][:], in0=sl[si][:], in1=g.oT[:, gate, cs],
                                                          op=ALU.mult),
                         r=[("sl", si), ("G", gate, tt)], w=[("slg", si)])
                rec.dve(lambda e, si=si, bb=bb, fi=fi: e.tensor_tensor(out=hid[hs][:, fi, :], in0=bb[:], in1=slg[si][:],
                                                                       op=ALU.mult),
                        r=[("psB", ai), ("slg", si)], w=[("hid", hs, fi)])

    def stage_out(n):
        gidx, tt = steps[n]
        s = gidx % NS
        cs = slice(tt * TT, (tt + 1) * TT)
        hs = n % 2
        for ec in range(DC):
            oi = cnt["o"] % 4
            cnt["o"] += 1
            bo = psO[oi]
            for fi in range(2):
                rec.pe(lambda e, fi=fi, ec=ec, bo=bo: e.matmul(bo[:], lhsT=w2g[s][:, fi, ec * 128:(ec + 1) * 128],
                                                               rhs=hid[hs][:, fi, :], start=(fi == 0), stop=(fi == 1)),
                       r=[("w2g", s), ("hid", hs, fi)], w=[("psO", oi)])
            rec.dve(lambda e, ec=ec, bo=bo: e.tensor_tensor(out=g.hT[:, ec, cs], in0=bo[:], in1=g.hT[:, ec, cs],
                                                            op=ALU.add),
                    r=[("psO", oi)], w=[("hT", ec, tt)])

    load(0)
    if len(groups) > 1 and NS > 2:
        load(1)
    for n in range(len(steps) + 1):
        if n < len(steps):
            gidx, tt = steps[n]
            if NS > 2:
                if tt == 1 and gidx + 2 < len(groups):
                    load(gidx + 2)
            else:
                if tt == 1 and gidx + 1 < len(groups):
                    load(gidx + 1)
            stage_ab(n)
        if n >= 1:
            stage_out(n - 1)


def phase_ffn(g, experts):
    from contextlib import ExitStack
    with ExitStack() as es:
        B = alloc_ffn_bufs(g, es, 3)
        phase(g, lambda rec: emit_ffn(g, rec, B, experts))


def phase_ffn_dense(g):
    phase_norm(g, 1)
    phase_ffn(g, [(g.w1, g.w3, g.w2, DFF, None)])


def phase_moe(g):
    phase_norm(g, 3, router=True)
    phase_moe_experts(g)


def emit_gbcast(g, rec, diag):
    G = g.oT
    gt = g.gt
    k = 0
    for ex in range(NE):
        for tt in range(NTT):
            def one(ex=ex, tt=tt, k=k):
                bank = g.ps[2 + k % 2]
                bn = ("psA", k % 2) if False else ("psB", k % 2)
                for j in range(4):
                    def sub(j=j):
                        tk = tt * 4 + j
                        dg = diag[(k * 4 + j) % len(diag)]
                        dn = ("diag", (k * 4 + j) % len(diag))
                        rec.dve(lambda e: e.tensor_scalar(out=dg[:], in0=g.ident_f[:], scalar1=gt[:, tk, ex:ex + 1],
                                                          scalar2=None, op0=ALU.mult), r=["consts"], w=[dn])
                        rec.pe(lambda e: e.matmul(bank[:, j * 128:(j + 1) * 128], lhsT=g.ones_f[:], rhs=dg[:], start=True, stop=True),
                               r=[dn, "consts"], w=[bn])
                    sub()
                rec.act(lambda e: e.activation(out=G[:, ex, tt * TT:(tt + 1) * TT], in_=bank[:], func=AF.Copy),
                        r=[bn], w=[("G", ex, tt)])
            one()
            k += 1


def emit_moe_sparse(g, rec, B, X):
    C = MOE_CAP
    NJ = C // 128
    CTS = [(0, 384), (384, C - 384)] if C > 384 else [(0, C)]
    hn_tok = g.oT.rearrange("p c (a b) -> p (c a) b", b=1024)
    hn_tok = g.oT[:].rearrange("p c s -> p (c s)").rearrange("p (t d) -> p t d", d=D)
    hnflat = g.hnT[:].rearrange("p c s -> p (c s)")
    Sel = hnflat[:, 0:16 * C].rearrange("p (t j) -> p t j", j=C)
    Xe = hnflat[:, 16 * C:16 * C + DC * C].rearrange("p (c j) -> p c j", j=C)
    Ybf = hnflat[:, 0:NJ * D].rearrange("p (j d) -> p j d", d=D)
    psbf = [g.ps[i][:].bitcast(BF16) for i in range(8)]
    w1g, w3g, w2g, hid, sl = B.w1g, B.w3g, B.w2g, B.hid, B.sl
    NS = B.NS
    rec.pool(lambda e: e.iota(X.iota_i, pattern=[[1, C]], base=0, channel_multiplier=0), w=["iota_i"])
    rec.dve(lambda e: e.tensor_copy(out=X.iotaC[:], in_=X.iota_i), r=["iota_i"], w=["iotaC"])
    rec.pool(lambda e: e.iota(X.jcol_i[:], pattern=[[128, NJ]], base=0, channel_multiplier=1), w=["jcol_i"])
    rec.dve(lambda e: e.tensor_copy(out=X.jcol[:], in_=X.jcol_i[:]), r=["jcol_i"], w=["jcol"])
    for tk in range(16):
        def tr(tk=tk):
            bi = tk % 2
            for c in range(DC):
                def t1(c=c):
                    rec.pe(lambda e: e.transpose(psbf[bi][:, c * 128:(c + 1) * 128], g.hnT[:, c, tk * 128:(tk + 1) * 128], g.ident_b[:]),
                           r=[("hnT", c, tk // 4), "consts"], w=[("psA", bi)])
                t1()
            if tk % 2 == 0:
                rec.act(lambda e: e.activation(out=hn_tok[:, tk, :], in_=psbf[bi][:, :], func=AF.Copy),
                        r=[("psA", bi)], w=[("hn_tok", tk)])
            else:
                rec.dve(lambda e: e.tensor_copy(out=hn_tok[:, tk, :], in_=psbf[bi][:, :]),
                        r=[("psA", bi)], w=[("hn_tok", tk)])
        tr()
    all_hn_tok = [("hn_tok", tk) for tk in range(16)]
    groups_all = []
    for ex in range(NE):
        for fg in range(DFE // 256):
            groups_all.append((g.e_w1[ex], g.e_w3[ex], g.e_w2[ex], fg, ex))
    NG = DFE // 256
    emit_w_load(g, rec, B, groups_all[0], 0)
    cnt = {"k": 0}

    def rot(name, n):
        v = cnt.get(name, 0)
        cnt[name] = v + 1
        return v % n

    def sel_ybf_overlap(tk, jj):
        return tk * C < (jj + 1) * D and jj * D < (tk + 1) * C

    def build_sel(ex, tks):
        for tk in tks:
            def mk(tk=tk):
                rec.dve(lambda e: e.tensor_scalar(out=Sel[:, tk, :], in0=X.iotaC[:], scalar1=g.rankm[:, tk, ex:ex + 1],
                                                  scalar2=None, op0=ALU.is_equal),
                        r=["iotaC"] + (all_hn_tok if ex == 0 else []),
                         w=[("Sel", tk)] + [("Ybf", jj) for jj in range(NJ) if sel_ybf_overlap(tk, jj)])
            mk()

    build_sel(0, range(16))
    for ex in range(NE):
        def expert(ex=ex):
            for c in range(DC):
                for (c0, cw) in CTS:
                    def ga(c=c, c0=c0, cw=cw):
                        bi = rot("psg", 2)
                        bank = g.ps[bi]
                        for tk in range(16):
                            def mm(tk=tk):
                                rec.pe(lambda e: e.matmul(bank[:, 0:cw], lhsT=hn_tok[:, tk, c * 128:(c + 1) * 128],
                                                          rhs=Sel[:, tk, c0:c0 + cw], start=(tk == 0), stop=(tk == 15)),
                                       r=[("hn_tok", tk), ("Sel", tk)], w=[("psA", bi)])
                            mm()
                        if (c + (c0 > 0)) % 2 == 0:
                            rec.act(lambda e: e.activation(out=Xe[:, c, c0:c0 + cw], in_=bank[:, 0:cw], func=AF.Copy),
                                    r=[("psA", bi)], w=[("Xe", c, c0)])
                        else:
                            rec.dve(lambda e: e.tensor_copy(out=Xe[:, c, c0:c0 + cw], in_=bank[:, 0:cw]),
                                    r=[("psA", bi)], w=[("Xe", c, c0)])
                    ga()
            gb = g.ps[2]
            for jt in range(NJ):
                for tk in range(16):
                    def gm_(jt=jt, tk=tk):
                        rec.pe(lambda e: e.matmul(gb[:, jt:jt + 1], lhsT=Sel[:, tk, jt * 128:(jt + 1) * 128],
                                                  rhs=g.gtb[:, tk, ex:ex + 1], start=(tk == 0), stop=(tk == 15)),
                               r=[("Sel", tk)], w=[("psB", 0)])
                    gm_()
            rec.dve(lambda e: e.tensor_copy(out=X.gcomp[:], in_=gb[:, 0:NJ]), r=[("psB", 0)], w=["gcomp"])
            prep_state = {}

            def prep(tt):
                ri = rot("rb", 2)
                rbank = g.ps[ri]
                for j4 in range(4):
                    def bc(j4=j4):
                        tk = tt * 4 + j4
                        di = rot("diag", 3)
                        rec.dve(lambda e: e.tensor_scalar(out=X.diag[di][:], in0=g.ident_f[:], scalar1=g.rankm[:, tk, ex:ex + 1],
                                                          scalar2=None, op0=ALU.mult), r=["consts"], w=[("diag", di)])
                        rec.pe(lambda e: e.matmul(rbank[:, j4 * 128:(j4 + 1) * 128], lhsT=g.ones_f[:], rhs=X.diag[di][:],
                                                  start=True, stop=True), r=[("diag", di), "consts"], w=[("psA", ri)])
                    bc()
                rbi = rot("RB", 1)
                rec.act(lambda e: e.activation(out=X.RB[rbi][:], in_=rbank[:], func=AF.Copy), r=[("psA", ri)], w=[("RB", rbi)])
                sti = rot("SelT", 2)
                SelT = X.SelT[sti]
                for jt in range(NJ):
                    def st(jt=jt):
                        rec.dve(lambda e: e.tensor_scalar(out=SelT[:, jt, :], in0=X.RB[rbi][:], scalar1=X.jcol[:, jt:jt + 1],
                                                          scalar2=None, op0=ALU.is_equal),
                                r=[("RB", rbi), "jcol"], w=[("SelT", sti, jt)])
                    st()
                prep_state[tt] = (sti, SelT)

            def yb(jt):
                rec.act(lambda e: e.activation(out=Ybf[:, jt, :], in_=X.Yacc[:, jt, :], func=AF.Copy, scale=X.gcomp[:, jt:jt + 1]),
                        r=[("Yacc", jt, 0), ("Yacc", jt, 1), "gcomp"],
                        w=[("Ybf", jt)] + [("Sel", tk) for tk in range(16) if sel_ybf_overlap(tk, jt)])

            for fg in range(NG):
                def fgroup(fg=fg):
                    gidx = ex * NG + fg
                    s = gidx % NS
                    if gidx + 1 < len(groups_all):
                        emit_w_load(g, rec, B, groups_all[gidx + 1], gidx + 1)
                    if fg == NG - 1:
                        prep(0)
                    if fg == NG - 5 and ex + 1 < NE:
                        build_sel(ex + 1, [tk for tk in range(16) if not any(sel_ybf_overlap(tk, jj) for jj in range(NJ))])
                    for (c0, cw) in CTS:
                        def abpart(c0=c0, cw=cw):
                            hs = rot("hid", 2)
                            for fi in range(2):
                                def ab(fi=fi):
                                    ai = rot("ab", 2)
                                    ba, bb = g.ps[ai], g.ps[2 + ai]
                                    for c in range(DC):
                                        def m1(c=c):
                                            rec.pe(lambda e: e.matmul(ba[:, 0:cw], lhsT=w1g[s][:, c, fi * 128:(fi + 1) * 128],
                                                                      rhs=Xe[:, c, c0:c0 + cw], start=(c == 0), stop=(c == DC - 1)),
                                                   r=[("w1g", s), ("Xe", c, c0)], w=[("psA", ai)])
                                        m1()
                                    for c in range(DC):
                                        def m3(c=c):
                                            rec.pe(lambda e: e.matmul(bb[:, 0:cw], lhsT=w3g[s][:, c, fi * 128:(fi + 1) * 128],
                                                                      rhs=Xe[:, c, c0:c0 + cw], start=(c == 0), stop=(c == DC - 1)),
                                                   r=[("w3g", s), ("Xe", c, c0)], w=[("psB", ai)])
                                        m3()
                                    si = rot("sl", 2)
                                    rec.act(lambda e: e.activation(out=sl[si][:, 0:cw], in_=ba[:, 0:cw], func=AF.Silu),
                                            r=[("psA", ai)], w=[("sl", si)])
                                    rec.dve(lambda e: e.tensor_tensor(out=hid[hs][:, fi, 0:cw], in0=bb[:, 0:cw], in1=sl[si][:, 0:cw],
                                                                      op=ALU.mult),
                                            r=[("psB", ai), ("sl", si)], w=[("hid", hs, fi)])
                                ab()
                            for jl in range(cw // 128):
                                jt = c0 // 128 + jl
                                for dh in range(2):
                                    def outp(jl=jl, jt=jt, dh=dh):
                                        oi = rot("o", 4)
                                        bo = g.ps[4 + oi]
                                        for fi in range(2):
                                            def mo(fi=fi):
                                                rec.pe(lambda e: e.matmul(bo[:], lhsT=hid[hs][:, fi, jl * 128:(jl + 1) * 128],
                                                                          rhs=w2g[s][:, fi, dh * 512:(dh + 1) * 512],
                                                                          start=(fi == 0), stop=(fi == 1)),
                                                       r=[("w2g", s), ("hid", hs, fi)], w=[("psO", oi)])
                                            mo()
                                        if fg == 0:
                                            rec.dve(lambda e: e.tensor_copy(out=X.Yacc[:, jt, dh * 512:(dh + 1) * 512], in_=bo[:]),
                                                    r=[("psO", oi)], w=[("Yacc", jt, dh)])
                                        else:
                                            rec.dve(lambda e: e.tensor_tensor(out=X.Yacc[:, jt, dh * 512:(dh + 1) * 512], in0=bo[:],
                                                                              in1=X.Yacc[:, jt, dh * 512:(dh + 1) * 512], op=ALU.add),
                                                    r=[("psO", oi)], w=[("Yacc", jt, dh)])
                                    outp()
                                if fg == NG - 1:
                                    yb(jt)
                        abpart()
                fgroup()
            for tt in range(NTT):
                def sc(tt=tt):
                    sti, SelT = prep_state[tt]
                    for dc in range(DC):
                        def so(dc=dc):
                            oi = rot("o", 4)
                            bo = g.ps[4 + oi]
                            for jt in range(NJ):
                                def mm(jt=jt):
                                    rec.pe(lambda e: e.matmul(bo[:], lhsT=Ybf[:, jt, dc * 128:(dc + 1) * 128], rhs=SelT[:, jt, :],
                                                              start=(jt == 0), stop=(jt == NJ - 1)),
                                           r=[("Ybf", jt), ("SelT", sti, jt)], w=[("psO", oi)])
                                mm()
                            rec.dve(lambda e: e.tensor_tensor(out=g.hT[:, dc, tt * TT:(tt + 1) * TT], in0=bo[:],
                                                              in1=g.hT[:, dc, tt * TT:(tt + 1) * TT], op=ALU.add),
                                    r=[("psO", oi)], w=[("hT", dc, tt)])
                            for _ in range(FILL_MOE):
                                rec.pe(lambda e: e.matmul(g.ps[3][:], lhsT=g.ident_b[:], rhs=hn_tok[:, 0, 0:TT], start=True, stop=True),
                                       r=["consts"], w=[("psB", 1)])
                        so()
                        if dc == 3 and tt + 1 < NTT:
                            prep(tt + 1)
                sc()
            if ex + 1 < NE:
                build_sel(ex + 1, [tk for tk in range(16) if any(sel_ybf_overlap(tk, jj) for jj in range(NJ))])
        expert()


class Rec2:
    pass


def phase_moe_experts(g):
    nc = g.nc
    from contextlib import ExitStack
    k = g.nphase
    g.nphase += 1
    with ExitStack() as es:
        g.nphase -= 1
        B = alloc_ffn_bufs(g, es, 2)
        T = lambda name, shape, dt: es.enter_context(nc.sbuf_tensor(f"{name}_p{k}", shape, dt))
        X = Ctx()
        C = MOE_CAP
        NJ = C // 128
        X.iotaC = T("iotaC", [128, C], F32)
        X.jcol_i = T("jcol_i", [128, NJ], mybir.dt.int32)
        X.jcol = T("jcol", [128, NJ], F32)
        X.gcomp = T("gcomp", [128, NJ], F32)
        X.Yacc = T("Yacc", [128, NJ, D], F32)
        X.iota_i = X.Yacc[:, 0, 0:C].bitcast(mybir.dt.int32)
        X.RB = [T(f"RB{i}", [128, TT], F32) for i in range(1)]
        X.SelT = [T(f"SelT{i}", [128, NJ, TT], BF16) for i in range(2)]
        X.diag = [T(f"diag{i}", [128, 128], F32) for i in range(3)]
        g.nphase += 1

        recD = Rec(nc, True)
        emit_gbcast(g, recD, X.diag)
        emit_ffn(g, recD, B, [(g.e_w1[ex], g.e_w3[ex], g.e_w2[ex], DFE, ex) for ex in range(NE)])
        recS = Rec(nc, True)
        emit_moe_sparse(g, recS, B, X)

        cnt0, dcnt0 = dict(g.cnt), dict(g.dcnt)
        cntD, dcntD = dict(cnt0), dict(dcnt0)
        cntS, dcntS = dict(cnt0), dict(dcnt0)
        perD = recD.emit(g.sems, g.dma_sems, cntD, dcntD)
        perS = recS.emit(g.sems, g.dma_sems, cntS, dcntS)
        for e in ENGS:
            g.cnt[e] = max(cntD[e], cntS[e])
            g.dcnt[e] = max(dcntD[e], dcntS[e])

        def dma_final(rec):
            final = {}
            for op in rec.ops:
                if op.dma:
                    final[(op.eng, id(op.sem))] = (op.sem, op.val)
            return final

        def dma_sem_val(q, j, n):
            kk = len(g.dma_sems[q])
            if kk == 0:
                return 0
            return 16 * ((n - j + kk - 1) // kk) if n > j else 0

        def branch(eng_name, eng, rec, per, cntX, dcntX):
            rec.run_engine(eng_name, eng, per)
            for (q, _), (s_, v) in dma_final(rec).items():
                if q == eng_name:
                    eng.wait_ge(s_, v)

        with nc.Block() as block:
            def mk(eng_name):
                def body(eng):
                    eng.wait_ge(g.bar, 5 * k)
                    reg = eng.alloc_register(f"ovf_{eng_name}")
                    eng.reg_load(reg, g.flag_i[0:1, 0:1])
                    with eng.If(eng.snap(reg) > 0):
                        branch(eng_name, eng, recD, perD, cntD, dcntD)
                    with eng.Else():
                        branch(eng_name, eng, recS, perS, cntS, dcntS)
                    eng.drain().then_inc(g.bar, 1)
                return body
            block.tensor(mk("pe"))
            block.vector(mk("dve"))
            block.scalar(mk("act"))
            block.gpsimd(mk("pool"))
            block.sync(mk("sp"))
        g.sems, g.dma_sems = g.sems2, g.dma_sems2
        g.cnt = {e: 0 for e in ENGS}
        g.dcnt = {e: 0 for e in ENGS}


def phase_final(g):
    nc = g.nc
    from contextlib import ExitStack
    with ExitStack() as es:
        T = lambda name, shape, dt: es.enter_context(nc.sbuf_tensor(f"{name}_p{g.nphase}", shape, dt))
        sq = [T(f"sq{i}", [128, TT], F32) for i in range(3)]
        lnv = [T(f"lnv{i}", [128, TT], F32) for i in range(2)]
        rstd = [T(f"rstd{i}", [128, TT], F32) for i in range(2)]
        yT = [T(f"yT{i}", [128, TT], F32) for i in range(3)]
        ystage = [T(f"ystage{i}", [128, 4, D], F32) for i in range(2)]

        def b(rec):
            yv = g.y.rearrange("(t j p) d -> t p j d", j=4, p=128)
            cnt = {"k": 0}

            def out_f32(rec, tt, c, gcol, r_, rs_n):
                cs = slice(tt * TT, (tt + 1) * TT)
                k = cnt["k"]
                cnt["k"] += 1
                yt = yT[k % 3]
                yn = ("yT", k % 3)
                bank = g.ps[2 + k % 4]
                bn = ("pst", k % 4)
                st = ystage[tt % 2]
                rec.dve(lambda e: e.scalar_tensor_tensor(out=yt[:], in0=g.hT[:, c, cs], scalar=gcol, in1=r_[:],
                                                         op0=ALU.mult, op1=ALU.mult),
                        r=[("hT", c, tt), rs_n, "consts"], w=[yn])
                for j in range(4):
                    rec.pe(lambda e, j=j: e.transpose(bank[:, j * 128:(j + 1) * 128], yt[:, j * 128:(j + 1) * 128], g.ident_f[:]),
                           r=[yn], w=[bn])
                rec.act(lambda e: e.activation(out=st[:, :, c * 128:(c + 1) * 128],
                                               in_=bank[:].rearrange("p (j d) -> p j d", j=4), func=AF.Copy),
                        r=[bn], w=[("ystage", tt % 2, c)])
                if c == DC - 1:
                    rec.dma("sp", lambda e: e.dma_start(out=yv[tt], in_=st[:]),
                            r=[("ystage", tt % 2, cc) for cc in range(DC)], w=[("ystage_out", tt % 2)])

            emit_rmsnorm(g, rec, 4, sq, lnv, rstd, [g.ps[0], g.ps[1]], out_bf=False, out_f32=out_f32)
        phase(g, b)


def phase_attn(g, layer):
    phase_norm(g, 2 * layer)
    dbg_o = getattr(g, "stop", None) == f"attn{layer}_o"
    if layer == 0:
        phase_attn_sb(g, fuse_wo=not dbg_o)
    else:
        phase_attn_moba(g, fuse_wo=not dbg_o)
    if dbg_o:
        def b(rec):
            for c in range(DC):
                def one(c=c):
                    rec.dve(lambda e: e.tensor_copy(out=g.hT[:, c, :], in_=g.oT[:, c, :]), w=[("hT", c)])
                one()
        phase(g, b)
        return


def _emit_qkv_weights(g, rec, layer, hp, wq, wk, wv):
    s = hp % 2
    wsrc = g.w_qkv[layer].rearrange("(c p) n -> p c n", p=128)
    for i, (wt, nm) in enumerate(((wq, "wq"), (wk, "wk"), (wv, "wv"))):
        def one(wt=wt, nm=nm, i=i):
            cols = slice(i * D + hp * 128, i * D + (hp + 1) * 128)
            rec.dma("pool", lambda e: e.dma_start(out=wt[s][:], in_=wsrc[:, :, cols]), w=[(nm, s)])
        one()


def _emit_fill(g, rec, psP, rotP, n):
    for _ in range(n):
        pi = rotP.next()
        bank = psP[pi]
        rec.pe(lambda e, bank=bank: e.matmul(bank[:], lhsT=g.ident_b[:], rhs=g.hnT[:, 0, 0:TT], start=True, stop=True),
               r=["consts"], w=[("psP", pi)])


def _emit_wo_load(g, rec, wo, layer):
    src = g.w_o[layer].rearrange("(c p) e -> p c e", p=128)
    for h2 in range(2):
        rec.dma("pool", lambda e, h2=h2: e.dma_start(out=wo[:, h2 * 4:(h2 + 1) * 4, :], in_=src[:, h2 * 4:(h2 + 1) * 4, :]),
                w=[("wo", h2)])


def _emit_wo(g, rec, wo, psP, rotP):
    for tt in range(NTT):
        for ec in range(DC):
            def grp(tt=tt, ec=ec):
                cs = slice(tt * TT, (tt + 1) * TT)
                pi = rotP.next()
                bank = psP[pi]
                for c in range(DC):
                    rec.pe(lambda e, c=c: e.matmul(bank[:], lhsT=wo[:, c, ec * 128:(ec + 1) * 128], rhs=g.oT[:, c, cs],
                                                   start=(c == 0), stop=(c == DC - 1)),
                           r=[("wo", c // 4), ("oT", c, tt, 0), ("oT", c, tt, 1)], w=[("psP", pi)])
                rec.dve(lambda e: e.tensor_tensor(out=g.hT[:, ec, cs], in0=bank[:], in1=g.hT[:, ec, cs], op=ALU.add),
                        r=[("psP", pi)], w=[("hT", ec, tt)])
            grp()


def _emit_v_proj(g, rec, hp, wv, V, psP, rotP):
    s = hp % 2
    for t4 in range(4):
        def one(t4=t4):
            pi = rotP.next()
            bank = psP[pi]
            for j in range(4):
                tok = t4 * 4 + j
                for c in range(DC):
                    def mm(j=j, tok=tok, c=c):
                        rec.pe(lambda e: e.matmul(bank[:, j * 128:(j + 1) * 128], lhsT=g.hnT[:, c, tok * 128:(tok + 1) * 128],
                                                  rhs=wv[s][:, c, :], start=(c == 0), stop=(c == DC - 1)),
                               r=[("wv", s), ("hnT", c, tok // 4)], w=[("psP", pi)])
                    mm()
            rec.dve(lambda e: e.tensor_copy(out=V[s][:, t4 * 4:(t4 + 1) * 4, :],
                                            in_=bank[:].rearrange("p (j n) -> p j n", j=4)),
                    r=[("psP", pi)], w=[("V", s, t4)])
        one()


def phase_attn_sb(g, fuse_wo=True):
    nc = g.nc
    from contextlib import ExitStack
    with ExitStack() as es:
        T = lambda name, shape, dt: es.enter_context(nc.sbuf_tensor(f"{name}_p{g.nphase}", shape, dt))
        wq = [T(f"wq{i}", [128, DC, 128], BF16) for i in range(2)]
        wk = [T(f"wk{i}", [128, DC, 128], BF16) for i in range(2)]
        wv = [T(f"wv{i}", [128, DC, 128], BF16) for i in range(2)]
        qm = [[T(f"qm{i}_{hh}", [128, S], BF16) for hh in range(2)] for i in range(2)]
        wo = T("wo", [128, DC, D], BF16) if fuse_wo else None
        kT = [T(f"kT{i}", [128, S], BF16) for i in range(2)]
        V = [T(f"V{i}", [128, 16, 128], BF16) for i in range(2)]
        NB = 3
        E = [T(f"E{i}", [128, TT], F32) for i in range(NB)]
        Lp = [T(f"Lp{i}", [128, TT], BF16) for i in range(NB)]
        Acc = [T(f"Acc{i}", [128, TT], BF16) for i in range(NB)]
        W = [T(f"W{i}", [128, TT], BF16) for i in range(NB)]
        psZ = [g.ps[0], g.ps[1]]
        psC = [g.ps[2], g.ps[3]]
        psO = [g.ps[4], g.ps[5]]
        psP = [g.ps[6], g.ps[7]]

        def b(rec):
            rotZ, rotC, rotO, rotP = Rot(2), Rot(2), Rot(2), Rot(2)
            rotE, rotL, rotA, rotW = Rot(NB), Rot(NB), Rot(NB), Rot(NB)

            def proj_ops(hp):
                ops = []
                s = hp % 2
                for tt in range(NTT):
                    for kind in ("q", "k"):
                        st = {}
                        for c in range(DC):
                            def mm(c=c, tt=tt, kind=kind, st=st):
                                if c == 0:
                                    st["pi"] = rotP.next()
                                pi = st["pi"]
                                bank = psP[pi]
                                wt, wn = (wq, "wq") if kind == "q" else (wk, "wk")
                                cs = slice(tt * TT, (tt + 1) * TT)
                                rec.pe(lambda e: e.matmul(bank[:], lhsT=wt[s][:, c, :], rhs=g.hnT[:, c, cs],
                                                          start=(c == 0), stop=(c == DC - 1)),
                                       r=[(wn, s), ("hnT", c, tt)], w=[("psP", pi)])
                                if c == DC - 1:
                                    if kind == "q":
                                        for hh in range(2):
                                            hb = hh * 64
                                            rec.dve(lambda e, hh=hh, hb=hb: e.tensor_scalar(
                                                out=qm[s][hh][hb:hb + 64, cs], in0=bank[hb:hb + 64, :], scalar1=DH ** -0.5,
                                                scalar2=None, op0=ALU.mult), r=[("psP", pi), ("qz", s, hh)], w=[("q", s, tt, hh)])
                                    else:
                                        rec.dve(lambda e: e.tensor_copy(out=kT[s][:, cs], in_=bank[:]),
                                                r=[("psP", pi)], w=[("k", s, tt)])
                            ops.append(mm)
                for t4 in range(4):
                    st = {}
                    for j in range(4):
                        for ch in range(2):
                            def vm(t4=t4, j=j, ch=ch, st=st):
                                if j == 0 and ch == 0:
                                    st["pi"] = rotP.next()
                                pi = st["pi"]
                                bank = psP[pi]
                                tok = t4 * 4 + j
                                for c in range(ch * 4, ch * 4 + 4):
                                    rec.pe(lambda e, c=c: e.matmul(bank[:, j * 128:(j + 1) * 128],
                                                                   lhsT=g.hnT[:, c, tok * 128:(tok + 1) * 128], rhs=wv[s][:, c, :],
                                                                   start=(c == 0), stop=(c == DC - 1)),
                                           r=[("wv", s), ("hnT", c, tok // 4)], w=[("psP", pi)])
                                if j == 3 and ch == 1:
                                    rec.dve(lambda e: e.tensor_copy(out=V[s][:, t4 * 4:(t4 + 1) * 4, :],
                                                                    in_=bank[:].rearrange("p (j n) -> p j n", j=4)),
                                            r=[("psP", pi)], w=[("V", s, t4)])
                            ops.append(vm)
                return ops

            def make_tiles(hp):
                tiles = []
                for hh in range(2):
                    for tt in range(NTT):
                        chain = []
                        for i, kb in enumerate(range(4 * tt + 3, -1, -1)):
                            j = kb - 4 * tt
                            t = Ctx()
                            t.hp, t.hh, t.tt, t.kb, t.i = hp, hh, tt, kb, i
                            t.c0 = 128 * j if j >= 0 else 0
                            t.diag = j >= 0
                            t.last = kb == 0
                            t.ain = None
                            chain.append(t)
                        for a, bb in zip(chain[:-1], chain[1:]):
                            a.nxt = bb
                        chain[-1].nxt = None
                        tiles += chain
                return tiles

            def qk_aps(t):
                s = t.hp % 2
                hb = t.hh * 64
                lhsT = kT[s][:, t.kb * 128:(t.kb + 1) * 128]
                rhs = qm[s][t.hh][:, t.tt * TT + t.c0:(t.tt + 1) * TT]
                deps = [("k", s, t.kb // 4), ("q", s, t.tt, t.hh)]
                return lhsT, rhs, deps

            def s1(t):
                c0 = t.c0
                lhsT, rhs, deps = qk_aps(t)
                zb = rotZ.next()
                rec.pe(lambda e: e.matmul(psZ[zb][:, c0:], lhsT=lhsT, rhs=rhs, start=True, stop=True),
                       r=deps, w=[("Z", zb)])
                eb = rotE.next()
                rec.act(lambda e: e.activation(out=E[eb][:, c0:], in_=psZ[zb][:, c0:], func=AF.Exp),
                        r=[("Z", zb)], w=[("E", eb)])
                lb = rotL.next()
                t.lb = lb
                rec.act(lambda e: e.activation(out=Lp[lb][:, c0:], in_=E[eb][:, c0:], func=AF.Ln, bias=1.0),
                        r=[("E", eb)], w=[("Lp", lb)])
                if t.diag:
                    rec.dve(lambda e: e.tensor_tensor(out=Lp[lb][:, c0:c0 + 128], in0=Lp[lb][:, c0:c0 + 128],
                                                      in1=g.mstrict[:], op=ALU.mult),
                            r=[("Lp", lb), "consts"], w=[("Lp", lb)])
                if t.nxt is not None:
                    ab = rotA.next()
                    t.nxt.ain = ab
                    c0n = t.nxt.c0
                    if c0n < c0:
                        rec.pool(lambda e: e.memset(Acc[ab][:, c0n:c0], 0.0), w=[("Acc", ab)])
                    if t.i == 0:
                        rec.pool(lambda e: e.tensor_copy(out=Acc[ab][:, c0:], in_=Lp[lb][:, c0:]),
                                 r=[("Lp", lb)], w=[("Acc", ab)])
                    else:
                        ain = t.ain
                        rec.pool(lambda e: e.tensor_tensor(out=Acc[ab][:, c0:], in0=Acc[ain][:, c0:], in1=Lp[lb][:, c0:],
                                                           op=ALU.add),
                                 r=[("Lp", lb), ("Acc", ain)], w=[("Acc", ab)])

            def s2(t):
                c0 = t.c0
                lb = t.lb
                lhsT, rhs, deps = qk_aps(t)
                cb = rotC.next()
                rec.pe(lambda e: e.matmul(psC[cb][:, c0:], lhsT=g.neguincl[:], rhs=Lp[lb][:, c0:], start=True, stop=False),
                       r=[("Lp", lb), "consts"], w=[("C", cb)])
                if t.i > 0:
                    ain = t.ain
                    rec.pe(lambda e: e.matmul(psC[cb][:, c0:], lhsT=g.negones_b[:], rhs=Acc[ain][:, c0:], start=False, stop=False),
                           r=[("Acc", ain), "consts"], w=[("C", cb)])
                rec.pe(lambda e: e.matmul(psC[cb][:, c0:], lhsT=lhsT, rhs=rhs, start=False, stop=True),
                       r=deps, w=[("C", cb)])
                wb = rotW.next()
                t.wb = wb
                rec.act(lambda e: e.activation(out=W[wb][:, c0:], in_=psC[cb][:, c0:], func=AF.Exp),
                        r=[("C", cb)], w=[("W", wb)])
                if t.diag:
                    rec.dve(lambda e: e.tensor_tensor(out=W[wb][:, c0:c0 + 128], in0=W[wb][:, c0:c0 + 128],
                                                      in1=g.mstrict[:], op=ALU.mult),
                            r=[("W", wb), "consts"], w=[("W", wb)])

            ostate = {}

            def s3(t):
                c0 = t.c0
                s = t.hp % 2
                hb = t.hh * 64
                M = 128
                if t.i == 0:
                    ob = rotO.next()
                    ostate["ob"] = ob
                    rec.pe(lambda e: e.matmul(psO[ob][0:M, :], lhsT=g.zeros_b[:, 0:M], rhs=g.ones_b[:, 0:1].to_broadcast([128, TT]) if False else g.hnT[:, 0, 0:TT],
                                              start=True, stop=False),
                           r=["consts"], w=[("O", ob)])
                ob = ostate["ob"]
                wb = t.wb
                kb = t.kb
                last = t.last
                rec.pe(lambda e: e.matmul(psO[ob][0:M, c0:], lhsT=V[s][:, kb, 0:M], rhs=W[wb][:, c0:], start=False, stop=last),
                       r=[("V", s, kb // 4), ("W", wb)], w=[("O", ob)])
                if last:
                    hp, tt = t.hp, t.tt
                    rec.dve(lambda e: e.tensor_copy(out=g.oT[hb:hb + 64, hp, tt * TT:(tt + 1) * TT], in_=psO[ob][hb:hb + 64, :]),
                            r=[("O", ob)], w=[("oT", hp, tt, t.hh)])

            _emit_qkv_weights(g, rec, 0, 0, wq, wk, wv)
            for i in range(2):
                def zq(i=i):
                    rec.pool(lambda e: e.memset(qm[i][0][64:128, :], 0.0), w=[("qz", i, 0)])
                    rec.pool(lambda e: e.memset(qm[i][1][0:64, :], 0.0), w=[("qz", i, 1)])
                zq()
            for op in proj_ops(0):
                op()
            for hp in range(DC):
                pending = []
                if hp + 1 < DC:
                    _emit_qkv_weights(g, rec, 0, hp + 1, wq, wk, wv)
                    pending = proj_ops(hp + 1)
                elif fuse_wo:
                    _emit_wo_load(g, rec, wo, 0)
                tiles = make_tiles(hp)
                n = len(tiles)
                for i in range(n + 2):
                    if i < n:
                        s1(tiles[i])
                    if 1 <= i <= n:
                        s2(tiles[i - 1])
                    if i >= 2:
                        s3(tiles[i - 2])
                    for _ in range(FILL_SB):
                        if pending and i >= FILL_DELAY:
                            pending.pop(0)()
                        else:
                            _emit_fill(g, rec, psP, rotP, 1)
                while pending:
                    pending.pop(0)()
            if fuse_wo:
                _emit_wo(g, rec, wo, psP, rotP)
        phase(g, b)


def phase_attn_moba(g, fuse_wo=True):
    nc = g.nc
    from contextlib import ExitStack
    with ExitStack() as es:
        T = lambda name, shape, dt: es.enter_context(nc.sbuf_tensor(f"{name}_p{g.nphase}", shape, dt))
        wq = [T(f"wq{i}", [128, DC, 128], BF16) for i in range(2)]
        wk = [T(f"wk{i}", [128, DC, 128], BF16) for i in range(2)]
        wv = [T(f"wv{i}", [128, DC, 128], BF16) for i in range(2)]
        wo = T("wo", [128, DC, D], BF16) if fuse_wo else None
        qa = [T(f"qa{i}", [128, S], BF16) for i in range(2)]
        ka = [T(f"ka{i}", [128, S], BF16) for i in range(2)]
        V = [T(f"V{i}", [128, 16, 128], BF16) for i in range(2)]
        NB = 4
        P = [T(f"P{i}", [128, TT], BF16) for i in range(NB)]
        lnd = [T(f"lnd{i}", [128, TT], F32) for i in range(2)]
        rden = [T(f"rden{i}", [128, TT], F32) for i in range(2)]
        R = T("R", [128, NH, 2, 128], BF16)
        bstage = [T(f"bstage{i}", [128, 2, 128], F32) for i in range(2)]
        b31 = T("b31", [128, NH], F32)
        nb31 = T("nb31", [128, NH], F32)
        ksum = [T(f"ksum{i}", [64, 8], F32) for i in range(2)]
        kmT = [T(f"kmT{i}", [64, 8], BF16) for i in range(2)]
        gm = [T(f"gm{i}", [128, 8], F32) for i in range(3)]
        m8 = [T(f"m8{i}", [128, 8], F32) for i in range(3)]
        nmpad = [T(f"nmpad{i}", [128, 72], BF16) for i in range(3)]
        nmtmp = [T(f"nmtmp{i}", [128, 8], F32) for i in range(3)]
        colmask = T("colmask", [128, 4, 8], F32)
        gall = [T(f"gall{i}", [128, 64], F32) for i in range(2)]
        psZ = [g.ps[0], g.ps[1]]
        psO = [g.ps[2], g.ps[3]]
        psD = [g.ps[4], g.ps[5]]
        psP = [g.ps[6], g.ps[7]]

        def b(rec):
            rotZ, rotO, rotP, rotPb, rotG = Rot(2), Rot(2), Rot(2), Rot(NB), Rot(3)
            _emit_qkv_weights(g, rec, 1, 0, wq, wk, wv)
            rec.dma("sp", lambda e: e.dma_start(out=b31[:], in_=g.b31), w=["b31"])
            rec.dve(lambda e: e.tensor_scalar(out=nb31[:], in0=b31[:], scalar1=-1.0, scalar2=None, op0=ALU.mult),
                    r=["b31"], w=["nb31"])
            for h in range(NH):
                def one(h=h):
                    bs = bstage[h % 2]
                    rec.dma("sp", lambda e: e.dma_start(out=bs[:], in_=g.biasT[h]), w=[("bstage", h % 2)])
                    rec.act(lambda e: e.activation(out=R[:, h, :, :], in_=bs[:], func=AF.Exp, bias=nb31[:, h:h + 1]),
                            r=[("bstage", h % 2), "nb31"], w=[("R", h)])
                one()
            for i in range(2):
                def one(i=i):
                    rec.pool(lambda e: e.memset(ka[i][64:128, :], 0.0), w=[("ka_oh", i)])
                    rec.pool(lambda e: e.memset(ka[i][64:72, :], 1.0), r=[("ka_oh", i)], w=[("ka_oh", i)])
                    rec.pool(lambda e: e.affine_select(out=ka[i][64:72, :], in_=ka[i][64:72, :], pattern=[[1, S]],
                                                       compare_op=ALU.is_ge, fill=0.0, base=0, channel_multiplier=-256),
                             r=[("ka_oh", i)], w=[("ka_oh", i)])
                    rec.pool(lambda e: e.affine_select(out=ka[i][64:72, :], in_=ka[i][64:72, :], pattern=[[-1, S]],
                                                       compare_op=ALU.is_ge, fill=0.0, base=255, channel_multiplier=256),
                             r=[("ka_oh", i)], w=[("ka_oh", i)])
                    rec.pool(lambda e: e.memset(qa[i][64:128, :], 0.0), w=[("qa_m0", i)])
                one()
            for i in range(3):
                def one(i=i):
                    rec.pool(lambda e: e.memset(nmpad[i][:, 0:64], 0.0), w=[("nmpad0", i)])
                one()
            rec.pool(lambda e: e.memset(colmask[:], 0.0), w=["colmask"])
            for o in range(4, 8):
                def cm(o=o):
                    rec.pool(lambda e: e.memset(colmask[:, o - 4, 0:o], 1.0), r=["colmask"], w=["colmask"])
                cm()

            def proj_head_ops(hp, hh):
                s = hp % 2
                v_ops, k_ops, q_ops = [], [], {tt: [] for tt in range(NTT)}
                if hh == 0:
                    for t4 in range(4):
                        st = {}
                        for j in range(4):
                            for ch in range(2):
                                def vm(t4=t4, j=j, ch=ch, st=st):
                                    if j == 0 and ch == 0:
                                        st["pi"] = rotP.next()
                                    pi = st["pi"]
                                    bank = psP[pi]
                                    tok = t4 * 4 + j
                                    for c in range(ch * 4, ch * 4 + 4):
                                        rec.pe(lambda e, c=c: e.matmul(bank[:, j * 128:(j + 1) * 128],
                                                                       lhsT=g.hnT[:, c, tok * 128:(tok + 1) * 128], rhs=wv[s][:, c, :],
                                                                       start=(c == 0), stop=(c == DC - 1)),
                                               r=[("wv", s), ("hnT", c, tok // 4)], w=[("psP", pi)])
                                    if j == 3 and ch == 1:
                                        rec.dve(lambda e: e.tensor_copy(out=V[s][:, t4 * 4:(t4 + 1) * 4, :],
                                                                        in_=bank[:].rearrange("p (j n) -> p j n", j=4)),
                                                r=[("psP", pi)], w=[("V", s, t4)])
                                v_ops.append(vm)
                for tt in range(NTT):
                    for kind in ("q", "k"):
                        st = {}
                        for c in range(DC):
                            def mm(c=c, tt=tt, kind=kind, st=st):
                                if c == 0:
                                    st["pi"] = rotP.next()
                                pi = st["pi"]
                                bank = psP[pi]
                                wt, wn, dst, dn = (wq, "wq", qa, "qa") if kind == "q" else (wk, "wk", ka, "ka")
                                cs = slice(tt * TT, (tt + 1) * TT)
                                rec.pe(lambda e: e.matmul(bank[0:64, :], lhsT=wt[s][:, c, hh * 64:(hh + 1) * 64],
                                                          rhs=g.hnT[:, c, cs], start=(c == 0), stop=(c == DC - 1)),
                                       r=[(wn, s), ("hnT", c, tt)], w=[("psP", pi)])
                                if c == DC - 1:
                                    if kind == "q":
                                        rec.dve(lambda e: e.tensor_scalar(out=dst[hh][0:64, cs], in0=bank[0:64, :], scalar1=DH ** -0.5,
                                                                          scalar2=None, op0=ALU.mult),
                                                r=[("psP", pi)], w=[(dn, hh, tt)])
                                    else:
                                        rec.dve(lambda e: e.tensor_copy(out=dst[hh][0:64, cs], in_=bank[0:64, :]),
                                                r=[("psP", pi)], w=[(dn, hh, tt)])
                            (q_ops[tt] if kind == "q" else k_ops).append(mm)
                junk = lambda: _emit_fill(g, rec, psP, rotP, 1)
                spacers = [q_ops[0], q_ops[1]] + [v_ops[i * 8:(i + 1) * 8] for i in range(len(v_ops) // 8)]
                ops = k_ops + q_ops[2] + q_ops[3]
                for item in gate_ops(hp, hh):
                    if item is None:
                        ops += spacers.pop(0) if spacers else [junk] * GATE_JUNK
                    else:
                        ops.append(item)
                for grp in spacers:
                    ops += grp
                return ops

            def gate_ops(hp, hh):
                st = {}

                def a1():
                    rec.dve(lambda e: e.tensor_reduce(out=ksum[hh][:], in_=ka[hh][0:64, :].rearrange("p (n k) -> p n k", k=256),
                                                      axis=mybir.AxisListType.X, op=ALU.add),
                            r=[("ka", hh, tt) for tt in range(NTT)], w=[("ksum", hh)])
                    rec.dve(lambda e: e.tensor_copy(out=kmT[hh][:], in_=ksum[hh][:]), r=[("ksum", hh)], w=[("kmT", hh)])

                def a2():
                    pi = rotP.next()
                    gbank = psP[pi]
                    for qt in range(8, 16):
                        def gmm(qt=qt):
                            rec.pe(lambda e: e.matmul(gbank[:, (qt - 8) * 8:(qt - 7) * 8], lhsT=qa[hh][0:64, qt * 128:(qt + 1) * 128],
                                                      rhs=kmT[hh][:], start=True, stop=True),
                                   r=[("qa", hh, qt // 4), ("kmT", hh)], w=[("psP", pi)])
                        gmm()
                    rec.dve(lambda e: e.tensor_copy(out=gall[hh][:], in_=gbank[:, 0:64]), r=[("psP", pi)], w=[("gall", hh)])

                def mk_b(qt):
                    def b_():
                        own = qt // 2
                        gi = rotG.next()
                        st[qt] = gi
                        rec.dve(lambda e: e.tensor_copy(out=gm[gi][:], in_=gall[hh][:, (qt - 8) * 8:(qt - 7) * 8]),
                                r=[("gall", hh)], w=[("gm", gi)])
                        rec.dve(lambda e: e.memset(gm[gi][:, own:8], -1e30), r=[("gm", gi)], w=[("gm", gi)])
                        rec.dve(lambda e: e.max(out=m8[gi][:], in_=gm[gi][:]), r=[("gm", gi)], w=[("m8", gi)])
                        rec.dve(lambda e: e.tensor_scalar(out=nmtmp[gi][:], in0=gm[gi][:], scalar1=m8[gi][:, 2:3],
                                                          scalar2=NEG, op0=ALU.is_lt, op1=ALU.mult),
                                r=[("gm", gi), ("m8", gi)], w=[("nmtmp", gi)])
                        rec.dve(lambda e: e.tensor_tensor(out=nmpad[gi][:, 64:72], in0=nmtmp[gi][:], in1=colmask[:, own - 4, :],
                                                          op=ALU.mult),
                                r=[("nmtmp", gi), "colmask"], w=[("nmpad", gi)])
                    return b_

                def mk_c(qt):
                    def c_():
                        gi = st[qt]
                        mi = rotP.next()
                        mbank = psP[mi]
                        rec.pe(lambda e: e.matmul(mbank[0:72, 0:128], lhsT=nmpad[gi][:, 0:72], rhs=g.ident_b[:],
                                                  start=True, stop=True),
                               r=[("nmpad", gi), ("nmpad0", gi), "consts"], w=[("psP", mi)])
                        rec.act(lambda e: e.activation(out=qa[hh][64:72, qt * 128:(qt + 1) * 128], in_=mbank[64:72, 0:128],
                                                       func=AF.Copy),
                                r=[("psP", mi), ("qa_m0", hh)], w=[("qa_m", hh, qt)])
                    return c_

                seq = [a1, None, a2, None, mk_b(8), mk_b(9), None, mk_c(8)]
                for qt in range(10, 16):
                    seq += [mk_b(qt), None, mk_c(qt - 1)]
                seq += [None, mk_c(15)]
                return seq

            def make_tiles(hp, hh):
                tiles = []
                for tt in range(NTT):
                    chain = []
                    for i, kb in enumerate(range(0, 4 * tt + 4)):
                        j = kb - 4 * tt
                        t = Ctx()
                        t.hp, t.hh, t.tt, t.kb, t.i = hp, hh, tt, kb, i
                        t.c0 = 128 * j if j >= 0 else 0
                        t.last = kb == 4 * tt + 3
                        chain.append(t)
                    tiles += chain
                return tiles

            ostate = {}

            def s1(t):
                c0 = t.c0
                hh, tt, kb = t.hh, t.tt, t.kb
                h = t.hp * 2 + hh
                zb = rotZ.next()
                deps = [("ka", hh, kb // 4), ("ka_oh", hh), ("qa", hh, tt), ("qa_m0", hh)]
                deps += [("qa_m", hh, qt) for qt in range(4 * tt, 4 * tt + 4) if qt >= 8]
                rec.pe(lambda e: e.matmul(psZ[zb][:, c0:], lhsT=ka[hh][:, kb * 128:(kb + 1) * 128],
                                          rhs=qa[hh][:, tt * TT + c0:(tt + 1) * TT], start=True, stop=True),
                       r=deps, w=[("Z", zb)])
                pb = rotPb.next()
                t.pb = pb
                rec.act(lambda e: e.activation(out=P[pb][:, c0:], in_=psZ[zb][:, c0:], func=AF.Exp, bias=b31[:, h:h + 1]),
                        r=[("Z", zb), "b31"], w=[("P", pb)])
                for qi in range(4):
                    delta = 4 * tt + qi - kb
                    if delta in (0, 1) and 128 * qi >= c0:
                        def fix(qi=qi, delta=delta):
                            cols = slice(128 * qi, 128 * (qi + 1))
                            rec.dve(lambda e: e.tensor_tensor(out=P[pb][:, cols], in0=P[pb][:, cols], in1=R[:, h, delta, :],
                                                              op=ALU.mult),
                                    r=[("P", pb), ("R", h)], w=[("P", pb)])
                        fix()

            def s2(t):
                c0 = t.c0
                hh, tt, kb, hp = t.hh, t.tt, t.kb, t.hp
                s = hp % 2
                hb = hh * 64
                M = 128
                first = t.i == 0
                if first:
                    assert c0 == 0
                    ob = rotO.next()
                    ostate["ob"] = ob
                ob = ostate["ob"]
                pb = t.pb
                last = t.last
                rec.pe(lambda e: e.matmul(psO[ob][0:M, c0:], lhsT=V[s][:, kb, 0:M], rhs=P[pb][:, c0:], start=first, stop=last),
                       r=[("V", s, kb // 4), ("P", pb)], w=[("O", ob)])
                rec.pe(lambda e: e.matmul(psD[ob][0:M, c0:], lhsT=g.ones_b[:, 0:M], rhs=P[pb][:, c0:], start=first, stop=last),
                       r=[("P", pb), "consts"], w=[("D", ob)])
                if last:
                    li = ob
                    rec.act(lambda e: e.activation(out=lnd[li][hb:hb + 64, :], in_=psD[ob][hb:hb + 64, :], func=AF.Ln),
                            r=[("D", ob)], w=[("lnd", li)])
                    rec.act(lambda e: e.activation(out=rden[li][hb:hb + 64, :], in_=lnd[li][hb:hb + 64, :], func=AF.Exp, scale=-1.0),
                            r=[("lnd", li)], w=[("rden", li)])
                    rec.dve(lambda e: e.tensor_tensor(out=g.oT[hb:hb + 64, hp, tt * TT:(tt + 1) * TT], in0=psO[ob][hb:hb + 64, :],
                                                      in1=rden[li][hb:hb + 64, :], op=ALU.mult),
                            r=[("O", ob), ("rden", li)], w=[("oT", hp, tt, hh)])

            for op in proj_head_ops(0, 0):
                op()
            for hp in range(DC):
                if hp + 1 < DC:
                    _emit_qkv_weights(g, rec, 1, hp + 1, wq, wk, wv)
                elif fuse_wo:
                    _emit_wo_load(g, rec, wo, 1)
                for hh in range(2):
                    pending = []
                    if hh == 0:
                        pending = proj_head_ops(hp, 1)
                    elif hp + 1 < DC:
                        pending = proj_head_ops(hp + 1, 0)
                    tiles = make_tiles(hp, hh)
                    n = len(tiles)
                    npull = max(FILL_MOBA, -(-len(pending) // (n - 2)))
                    for i in range(n + 2):
                        if i < n:
                            s1(tiles[i])
                        if i >= 2:
                            s2(tiles[i - 2])
                        for k_ in range(npull):
                            if pending:
                                pending.pop(0)()
                            elif k_ < FILL_MOBA:
                                _emit_fill(g, rec, psP, rotP, 1)
                    while pending:
                        pending.pop(0)()
            if fuse_wo:
                _emit_wo(g, rec, wo, psP, rotP)
        phase(g, b)


_NC_CACHE = {}


def kernel(x, w_qkv, w_o, mixer_norm, ffn_norm, rel_bias, w1, w3, w2, router, e_w1, e_w3, e_w2, final_norm):
    inp = dict(x=np.asarray(x), w_qkv=np.asarray(w_qkv), w_o=np.asarray(w_o), mixer_norm=np.asarray(mixer_norm),
               ffn_norm=np.asarray(ffn_norm), rel_bias=np.asarray(rel_bias), w1=np.asarray(w1), w3=np.asarray(w3),
               w2=np.asarray(w2), router=np.asarray(router), e_w1=np.asarray(e_w1), e_w3=np.asarray(e_w3),
               e_w2=np.asarray(e_w2), final_norm=np.asarray(final_norm))
    n = inp["x"].shape[0]
    if "nc" not in _NC_CACHE:
        _NC_CACHE["nc"] = build_nc()
    nc = _NC_CACHE["nc"]
    in_maps = [make_in_map(inp, b) for b in range(n)]
    res = run_bass_kernel_spmd(nc, in_maps, core_ids=list(range(n)))
    return np.stack([np.asarray(r["y"]) for r in res.results], axis=0).astype(np.float32)
```

```python
import numpy as np
import concourse.bass as bass
import concourse.mybir as mybir
from concourse.bass_utils import run_bass_kernel_spmd

F32 = mybir.dt.float32
BF16 = mybir.dt.bfloat16
AF = mybir.ActivationFunctionType
ALU = mybir.AluOpType

ENGS = ("pe", "dve", "act", "pool", "sp")
N_DMA_SEMS = 6


class Op:
    __slots__ = ("eng", "fn", "deps", "dma", "sig", "sem", "val", "idx", "prewait")

    def __init__(self, eng, fn, dma):
        self.eng = eng
        self.fn = fn
        self.dma = dma
        self.deps = []
        self.sig = False
        self.sem = None
        self.val = 0
        self.prewait = None


class Rec:
    def __init__(self, nc, same_engine_sync=True):
        self.nc = nc
        self.ops = []
        self.last_w = {}
        self.readers = {}
        self.same_engine_sync = same_engine_sync

    def add(self, eng, fn, r=(), w=(), dma=False):
        op = Op(eng, fn, dma)
        op.idx = len(self.ops)
        deps = set()
        for b in r:
            if b in self.last_w:
                deps.add(self.last_w[b])
        for b in w:
            if b in self.last_w:
                deps.add(self.last_w[b])
            for x in self.readers.get(b, ()):
                deps.add(x)
        deps.discard(op.idx)
        op.deps = sorted(deps)
        for b in r:
            self.readers.setdefault(b, []).append(op.idx)
        for b in w:
            self.last_w[b] = op.idx
            self.readers[b] = []
        self.ops.append(op)
        return op

    def pe(self, fn, r=(), w=()):
        return self.add("pe", fn, r, w)

    def dve(self, fn, r=(), w=()):
        return self.add("dve", fn, r, w)

    def act(self, fn, r=(), w=()):
        return self.add("act", fn, r, w)

    def pool(self, fn, r=(), w=()):
        return self.add("pool", fn, r, w)

    def dma(self, q, fn, r=(), w=()):
        return self.add(q, fn, r, w, dma=True)

    def _needs_wait(self, op, d):
        if d.dma:
            return True
        if d.eng != op.eng:
            return True
        if op.dma:
            return True
        if op.eng == "pe":
            return False
        return self.same_engine_sync

    def emit(self, sems, dma_sems, cnt=None, dcnt=None):
        nc = self.nc
        ops = self.ops
        for op in ops:
            for di in op.deps:
                d = ops[di]
                if self._needs_wait(op, d):
                    d.sig = True
        if cnt is None:
            cnt = {e: 0 for e in ENGS}
        if dcnt is None:
            dcnt = {e: 0 for e in ENGS}
        for op in ops:
            if op.dma:
                i = dcnt[op.eng]
                dcnt[op.eng] += 1
                k = len(dma_sems[op.eng])
                op.sem = dma_sems[op.eng][i % k]
                op.val = 16 * (i // k + 1)
                if i >= k:
                    op.prewait = (op.sem, 16 * (i // k))
            elif op.sig:
                cnt[op.eng] += 1
                op.sem = sems[op.eng]
                op.val = cnt[op.eng]
        per_eng = {e: [] for e in ENGS}
        for op in ops:
            per_eng[op.eng].append(op)
        return per_eng

    def run_engine(self, eng_name, eng, per_eng):
        ops = self.ops
        waited = {}
        for op in per_eng[eng_name]:
            waits = {}
            if op.prewait is not None:
                s, v = op.prewait
                waits[id(s)] = (s, v)
            for di in op.deps:
                d = ops[di]
                if not self._needs_wait(op, d):
                    continue
                key = id(d.sem)
                if key not in waits or waits[key][1] < d.val:
                    waits[key] = (d.sem, d.val)
            for key, (s, v) in waits.items():
                if waited.get(key, 0) >= v:
                    continue
                eng.wait_ge(s, v)
                waited[key] = v
            ins = op.fn(eng)
            if op.dma:
                ins.then_inc(op.sem, 16)
            elif op.sig:
                ins.then_inc(op.sem, 1)


def run_phase(nc, build, same_engine_sync=True, final_waits=True):
    rec = Rec(nc, same_engine_sync)
    from contextlib import ExitStack

    with ExitStack() as es:
        sems = {e: es.enter_context(nc.semaphore(f"s_{e}_{id(rec) % 100000}")) for e in ENGS}
        dma_sems = {
            e: [es.enter_context(nc.semaphore(f"d_{e}{i}_{id(rec) % 100000}")) for i in range(N_DMA_SEMS)]
            for e in ("sp", "pool", "act")
        }
        dma_sems["pe"] = []
        dma_sems["dve"] = []
        build(rec)
        per_eng = rec.emit(sems, dma_sems)
        final = {}
        for op in rec.ops:
            if op.dma:
                final[(op.eng, id(op.sem))] = (op.sem, op.val)
        with nc.Block() as block:

            def mk(eng_name):
                def body(eng):
                    rec.run_engine(eng_name, eng, per_eng)
                    for (q, _), (s, v) in final.items():
                        if q == eng_name:
                            eng.wait_ge(s, v)

                return body

            block.tensor(mk("pe"))
            block.vector(mk("dve"))
            block.scalar(mk("act"))
            block.gpsimd(mk("pool"))
            block.sync(mk("sp"))
    return rec


S = 2048
D = 1024
NH = 16
DH = 64
DC = 8
NTT = 4
TT = 512
DFF = 2816
DFE = 3584
NE = 8
EPS = 1e-6
NEG = -30000.0
FILL_SB = 1
FILL_MOBA = 0
FILL_MOE = 0
NORM_FILL = 0
GATE_JUNK = 2
FILL_DELAY = 10
MOE_CAP = 640
N_BUCKETS = 32
MAX_DISTANCE = 128


def _t5_bucket_np(dist):
    import math
    n = np.maximum(dist, 0)
    max_exact = N_BUCKETS // 2
    nf = np.maximum(n, 1).astype(np.float32)
    large = max_exact + (np.log(nf / np.float32(max_exact)) / np.float32(math.log(MAX_DISTANCE / max_exact))
                         * np.float32(N_BUCKETS - max_exact)).astype(np.int32)
    large = np.minimum(large, N_BUCKETS - 1)
    return np.where(n < max_exact, n, large)


class Ctx:
    pass


def build_nc(stop_after=None, dbg=False):
    from contextlib import ExitStack

    nc = bass.Bass("TRN2", target_bir_lowering=False)
    g = Ctx()
    g.nc = nc
    dram = lambda name, shape, kind="ExternalInput", dt=F32: nc.dram_tensor(name, shape, dt, kind=kind).ap()
    g.x = dram("x", [S, D])
    g.w_qkv = dram("w_qkv", [2, D, 3 * D])
    g.w_o = dram("w_o", [2, D, D])
    g.gains = dram("gains", [128, 5 * DC])
    g.biasT = dram("biasT", [NH, 128, 2, 128])
    g.b31 = dram("b31", [128, NH])
    g.w1 = dram("w1", [D, DFF])
    g.w3 = dram("w3", [D, DFF])
    g.w2 = dram("w2", [DFF, D])
    g.router = dram("router", [D, NE])
    g.e_w1 = dram("e_w1", [NE, D, DFE])
    g.e_w3 = dram("e_w3", [NE, D, DFE])
    g.e_w2 = dram("e_w2", [NE, DFE, D])
    g.y = dram("y", [S, D], kind="ExternalOutput")
    if dbg:
        g.dbg = dram("dbg", [128, DC, S], kind="ExternalOutput")

    with ExitStack() as es:
        T = lambda name, shape, dt: es.enter_context(nc.sbuf_tensor(name, shape, dt))
        g.hT = T("hT", [128, DC, S], F32)
        g.hnT = T("hnT", [128, DC, S], BF16)
        g.oT = T("oT", [128, DC, S], BF16)
        g.gains_sb = T("gains_sb", [128, 5 * DC], F32)
        g.ident_f = T("ident_f", [128, 128], F32)
        g.ident_b = T("ident_b", [128, 128], BF16)
        g.ones_f = T("ones_f", [128, 128], F32)
        g.ones_b = T("ones_b", [128, 128], BF16)
        g.negones_b = T("negones_b", [128, 128], BF16)
        g.zeros_b = T("zeros_b", [128, 128], BF16)
        g.neguincl = T("neguincl", [128, 128], BF16)
        g.mstrict = T("mstrict", [128, 128], BF16)
        g.gt = T("gt_g", [128, 16, NE], F32)
        g.gtb = T("gtb_g", [128, 16, NE], BF16)
        g.rankm = T("rankm_g", [128, 16, NE], F32)
        g.flag_i = T("flag_i", [128, 2], mybir.dt.int32)
        g.ps = [es.enter_context(nc.psum_tensor(f"ps{i}", [128, 512], F32)) for i in range(8)]
        g.bar = es.enter_context(nc.semaphore("bar"))
        g.nphase = 0
        g.sems = {e: es.enter_context(nc.semaphore(f"s_{e}")) for e in ENGS}
        g.dma_sems = {e: [es.enter_context(nc.semaphore(f"d_{e}{i}")) for i in range(N_DMA_SEMS)]
                      for e in ("sp", "pool", "act")}
        g.dma_sems["pe"] = []
        g.dma_sems["dve"] = []
        g.sems2 = {e: es.enter_context(nc.semaphore(f"t_{e}")) for e in ENGS}
        g.dma_sems2 = {e: [es.enter_context(nc.semaphore(f"u_{e}{i}")) for i in range(N_DMA_SEMS)]
                       for e in ("sp", "pool", "act")}
        g.dma_sems2["pe"] = []
        g.dma_sems2["dve"] = []
        g.cnt = {e: 0 for e in ENGS}
        g.dcnt = {e: 0 for e in ENGS}

        phases = [
            ("init", lambda g: phase_init(g)),
            ("attn0", lambda g: phase_attn(g, 0)),
            ("ffn0", lambda g: phase_ffn_dense(g)),
            ("attn1", lambda g: phase_attn(g, 1)),
            ("moe", lambda g: phase_moe(g)),
            ("final", lambda g: phase_final(g)),
        ]
        g.stop = stop_after
        for name, fn in phases:
            fn(g)
            if stop_after == name or (stop_after or "").startswith(name + "_"):
                break
        if dbg:
            def b(rec):
                rec.dma("sp", lambda e: e.dma_start(out=g.dbg, in_=g.hT[:]), r=["hT_all"])
            phase(g, b)
    return nc


def phase(g, build):
    nc = g.nc
    k = g.nphase
    g.nphase += 1
    rec = Rec(nc, True)
    from contextlib import ExitStack

    if True:
        sems, dma_sems = g.sems, g.dma_sems
        build(rec)
        per_eng = rec.emit(sems, dma_sems, g.cnt, g.dcnt)
        final = {}
        for op in rec.ops:
            if op.dma:
                final[(op.eng, id(op.sem))] = (op.sem, op.val)
        with nc.Block() as block:
            def mk(eng_name):
                def body(eng):
                    if k > 0:
                        eng.wait_ge(g.bar, 5 * k)
                    rec.run_engine(eng_name, eng, per_eng)
                    for (q, _), (s, v) in final.items():
                        if q == eng_name:
                            eng.wait_ge(s, v)
                    eng.drain().then_inc(g.bar, 1)
                return body
            block.tensor(mk("pe"))
            block.vector(mk("dve"))
            block.scalar(mk("act"))
            block.gpsimd(mk("pool"))
            block.sync(mk("sp"))
    return rec


class Rot:
    def __init__(self, n):
        self.n = n
        self.i = -1

    def next(self):
        self.i = (self.i + 1) % self.n
        return self.i


def emit_rmsnorm(g, rec, norm_idx, sq, lnv, rstd, psbanks, out_bf=True, out_f32=None):
    def sq_part(tt):
        cs = slice(tt * TT, (tt + 1) * TT)
        bank = psbanks[tt % len(psbanks)]
        bname = ("psn", tt % len(psbanks))

        def sqmm(c):
            s = sq[c % len(sq)]
            sn = ("sq", c % len(sq))
            rec.act(lambda e: e.activation(out=s[:], in_=g.hT[:, c, cs], func=AF.Square),
                    r=[("hT", c, tt)], w=[sn])
            rec.pe(lambda e: e.matmul(bank[:], lhsT=g.ones_b[:], rhs=s[:], start=(c == 0), stop=(c == DC - 1)),
                   r=[sn, "consts"], w=[bname])
            for _ in range(NORM_FILL):
                rec.pe(lambda e: e.matmul(g.ps[7][:], lhsT=g.ident_b[:], rhs=g.oT[:, 0, 0:TT], start=True, stop=True),
                       r=["consts"], w=["ps_junk"])
        for c in range(DC):
            sqmm(c)

    def out_part(tt):
        cs = slice(tt * TT, (tt + 1) * TT)
        bank = psbanks[tt % len(psbanks)]
        bname = ("psn", tt % len(psbanks))
        l = lnv[tt % len(lnv)]
        r_ = rstd[tt % len(rstd)]
        ln_n = ("lnv", tt % len(lnv))
        rs_n = ("rstd", tt % len(rstd))
        rec.act(lambda e: e.activation(out=l[:], in_=bank[:], func=AF.Ln, scale=1.0 / D, bias=EPS),
                r=[bname, "consts"], w=[ln_n])
        rec.act(lambda e: e.activation(out=r_[:], in_=l[:], func=AF.Exp, scale=-0.5),
                r=[ln_n], w=[rs_n])

        def outc(c):
            gcol = g.gains_sb[:, norm_idx * DC + c: norm_idx * DC + c + 1]
            if out_bf:
                rec.dve(lambda e: e.scalar_tensor_tensor(
                    out=g.hnT[:, c, cs], in0=g.hT[:, c, cs], scalar=gcol, in1=r_[:], op0=ALU.mult, op1=ALU.mult),
                    r=[("hT", c, tt), rs_n, "consts"], w=[("hnT", c, tt)])
            if out_f32 is not None:
                out_f32(rec, tt, c, gcol, r_, rs_n)
        for c in range(DC):
            outc(c)
    for tt in range(NTT + 1):
        if tt < NTT:
            sq_part(tt)
        if tt >= 1:
            out_part(tt - 1)


def phase_init(g):
    nc = g.nc
    from contextlib import ExitStack
    with ExitStack() as es:
        T = lambda name, shape, dt: es.enter_context(nc.sbuf_tensor(f"{name}_p{g.nphase}", shape, dt))
        stage = [T(f"xstage{i}", [128, 4, D], F32) for i in range(2)]

        def b(rec):
            rec.dma("sp", lambda e: e.dma_start(out=g.gains_sb[:], in_=g.gains), w=["consts_g"])
            rec.pool(lambda e: e.memset(g.ident_f[:], 0.0), w=["ident_f"])
            rec.pool(lambda e: e.affine_select(out=g.ident_f[:], in_=g.ident_f[:], pattern=[[1, 128]],
                                               compare_op=ALU.not_equal, fill=1.0, base=0, channel_multiplier=-1),
                     r=["ident_f"], w=["ident_f"])
            rec.pool(lambda e: e.tensor_copy(out=g.ident_b[:], in_=g.ident_f[:]), r=["ident_f"], w=["c1"])
            rec.pool(lambda e: e.memset(g.ones_f[:], 1.0), w=["c2"])
            rec.pool(lambda e: e.memset(g.ones_b[:], 1.0), w=["c3"])
            rec.pool(lambda e: e.memset(g.negones_b[:], -1.0), w=["c4"])
            rec.pool(lambda e: e.memset(g.zeros_b[:], 0.0), w=["c5"])
            rec.pool(lambda e: e.memset(g.neguincl[:], -1.0), w=["c6"])
            rec.pool(lambda e: e.affine_select(out=g.neguincl[:], in_=g.neguincl[:], pattern=[[-1, 128]],
                                               compare_op=ALU.is_ge, fill=0.0, base=0, channel_multiplier=1),
                     r=["c6"], w=["c6"])
            rec.pool(lambda e: e.memset(g.mstrict[:], 1.0), w=["c7"])
            rec.pool(lambda e: e.affine_select(out=g.mstrict[:], in_=g.mstrict[:], pattern=[[1, 128]],
                                               compare_op=ALU.is_gt, fill=0.0, base=0, channel_multiplier=-1),
                     r=["c7"], w=["c7"])
            xv = g.x.rearrange("(t j p) d -> t p j d", j=4, p=128)
            k = 0
            for tt in range(NTT):
                st = stage[tt % 2]
                sn = ("stage", tt % 2)
                rec.dma("sp", lambda e, st=st, tt=tt: e.dma_start(out=st[:], in_=xv[tt]), w=[sn])
                for c in range(DC):
                    bank = g.ps[k % 4]
                    bn = ("ps", k % 4)
                    for j in range(4):
                        rec.pe(lambda e, st=st, j=j, c=c, bank=bank: e.transpose(
                            bank[:, j * 128:(j + 1) * 128], st[:, j, c * 128:(c + 1) * 128], g.ident_f[:]),
                            r=[sn, "ident_f"], w=[bn])
                    dst = g.hT[:, c, tt * TT:(tt + 1) * TT]
                    if k % 2 == 0:
                        rec.act(lambda e, dst=dst, bank=bank: e.activation(out=dst, in_=bank[:], func=AF.Copy),
                                r=[bn], w=[("hT", c, tt)])
                    else:
                        rec.dve(lambda e, dst=dst, bank=bank: e.tensor_copy(out=dst, in_=bank[:]),
                                r=[bn], w=[("hT", c, tt)])
                    k += 1
        phase(g, b)


def make_in_map(inp, b):
    norms = np.stack([inp["mixer_norm"][0], inp["ffn_norm"][0], inp["mixer_norm"][1],
                      inp["ffn_norm"][1], inp["final_norm"]], axis=0)
    gains = np.ascontiguousarray(norms.reshape(5, DC, 128).transpose(2, 0, 1).reshape(128, 5 * DC))
    kk = np.arange(128)[:, None, None]
    jj = np.arange(2)[None, :, None]
    qq = np.arange(128)[None, None, :]
    dist = qq - kk + 128 * jj
    bt = inp["rel_bias"][_t5_bucket_np(dist), :]
    bt = np.where((dist >= 0)[..., None], bt, np.float32(-1e30)).astype(np.float32)
    biasT = np.ascontiguousarray(bt.transpose(3, 0, 1, 2))
    b31 = np.ascontiguousarray(np.broadcast_to(inp["rel_bias"][N_BUCKETS - 1][None, :], (128, NH))).astype(np.float32)
    return {
        "x": np.ascontiguousarray(inp["x"][b]),
        "w_qkv": inp["w_qkv"], "w_o": inp["w_o"], "gains": gains, "biasT": biasT, "b31": b31,
        "w1": inp["w1"][0], "w3": inp["w3"][0], "w2": inp["w2"][0], "router": inp["router"][0],
        "e_w1": inp["e_w1"][0], "e_w3": inp["e_w3"][0], "e_w2": inp["e_w2"][0],
    }


def phase_norm(g, norm_idx, router=False):
    nc = g.nc
    from contextlib import ExitStack
    with ExitStack() as es:
        T = lambda name, shape, dt: es.enter_context(nc.sbuf_tensor(f"{name}_p{g.nphase}", shape, dt))
        sq = [T(f"sq{i}", [128, TT], BF16) for i in range(4)]
        lnv = [T(f"lnv{i}", [128, TT], F32) for i in range(2)]
        rstd = [T(f"rstd{i}", [128, TT], F32) for i in range(2)]
        if router:
            hn32 = T("hn32", [128, DC, TT], F32)
            wr = T("wr", [128, DC, NE], F32)
            lg = T("lg", [128, 16, NE], F32)
            m8 = T("m8", [128, 16, 8], F32)
            sm = T("sm", [128, 16, 4], F32)
            t1 = T("t1", [128, NE], F32)
            t2 = T("t2", [128, NE], F32)
            gt = g.gt
            mf = T("mf", [128, 16, NE], F32)
            mb = T("mb", [128, 16, NE], BF16)
            macc = T("macc", [128, 17, NE], F32)
            maccb = T("maccb", [128, 17, NE], BF16)
            rtmp = T("rtmp", [128, 16, NE], F32)
            cmax = T("cmax", [128, 2], F32)

        def b(rec):
            out_f32 = None
            if router:
                rec.dma("sp", lambda e: e.dma_start(out=wr[:], in_=g.router.rearrange("(c p) e -> p c e", p=128)),
                        w=["wr"])

                def out_f32(rec, tt, c, gcol, r_, rs_n):
                    cs = slice(tt * TT, (tt + 1) * TT)
                    rec.dve(lambda e: e.scalar_tensor_tensor(
                        out=hn32[:, c, :], in0=g.hT[:, c, cs], scalar=gcol, in1=r_[:], op0=ALU.mult, op1=ALU.mult),
                        r=[("hT", c, tt), rs_n, "consts"], w=[("hn32", c)])
                    if c == DC - 1:
                        bank = g.ps[4 + tt % 2]
                        bn = ("psr", tt % 2)
                        for j in range(4):
                            for cc in range(DC):
                                rec.pe(lambda e, j=j, cc=cc: e.matmul(
                                    bank[:, j * 8:(j + 1) * 8], lhsT=hn32[:, cc, j * 128:(j + 1) * 128], rhs=wr[:, cc, :],
                                    start=(cc == 0), stop=(cc == DC - 1)),
                                    r=[("hn32", cc), "wr"], w=[bn])
                        rec.dve(lambda e: e.tensor_copy(out=lg[:, tt * 4:(tt + 1) * 4, :],
                                                        in_=bank[:, 0:32].rearrange("p (j e) -> p j e", j=4)),
                                r=[bn], w=[("lg", tt)])

            emit_rmsnorm(g, rec, norm_idx, sq, lnv, rstd, [g.ps[0], g.ps[1]], out_bf=True, out_f32=out_f32)
            if router:
                G = g.oT
                for tk in range(16):
                    tt = tk // 4
                    rec.dve(lambda e, tk=tk: e.max(out=m8[:, tk, :], in_=lg[:, tk, :]), r=[("lg", tt)], w=[("m8", tk)])
                    rec.dve(lambda e, tk=tk: e.tensor_tensor(out=sm[:, tk, 0:1], in0=m8[:, tk, 0:1], in1=m8[:, tk, 1:2],
                                                             op=ALU.subtract), r=[("m8", tk)], w=[("sm0", tk)])
                    rec.act(lambda e, tk=tk: e.activation(out=sm[:, tk, 1:2], in_=sm[:, tk, 0:1], func=AF.Exp, scale=-1.0),
                            r=[("sm0", tk)], w=[("sm1", tk)])
                    rec.dve(lambda e, tk=tk: e.tensor_scalar(out=sm[:, tk, 2:3], in0=sm[:, tk, 1:2], scalar1=1.0, scalar2=None,
                                                             op0=ALU.add), r=[("sm1", tk)], w=[("sm2", tk)])
                    rec.dve(lambda e, tk=tk: e.reciprocal(out=sm[:, tk, 2:3], in_=sm[:, tk, 2:3]),
                            r=[("sm2", tk)], w=[("sm2", tk)])
                    rec.dve(lambda e, tk=tk: e.tensor_tensor(out=sm[:, tk, 3:4], in0=sm[:, tk, 1:2], in1=sm[:, tk, 2:3],
                                                             op=ALU.mult), r=[("sm1", tk), ("sm2", tk)], w=[("sm3", tk)])
                    rec.dve(lambda e, tk=tk: e.tensor_scalar(out=t1[:], in0=lg[:, tk, :], scalar1=m8[:, tk, 0:1],
                                                             scalar2=sm[:, tk, 2:3], op0=ALU.is_equal, op1=ALU.mult),
                            r=[("lg", tt), ("m8", tk), ("sm2", tk)], w=["t1"])
                    rec.dve(lambda e, tk=tk: e.tensor_scalar(out=t2[:], in0=lg[:, tk, :], scalar1=m8[:, tk, 1:2],
                                                             scalar2=sm[:, tk, 3:4], op0=ALU.is_equal, op1=ALU.mult),
                            r=[("lg", tt), ("m8", tk), ("sm3", tk)], w=["t2"])
                    rec.dve(lambda e, tk=tk: e.tensor_tensor(out=gt[:, tk, :], in0=t1[:], in1=t2[:], op=ALU.add),
                            r=["t1", "t2"], w=[("gt", tk)])
                rec.dve(lambda e: e.tensor_copy(out=g.gtb[:], in_=gt[:]), r=[("gt", tk) for tk in range(16)], w=["gtb"])
                rec.dve(lambda e: e.tensor_scalar(out=mf[:], in0=gt[:], scalar1=0.0, scalar2=None, op0=ALU.is_gt),
                        r=[("gt", tk) for tk in range(16)], w=["mf"])
                rec.dve(lambda e: e.tensor_copy(out=mb[:], in_=mf[:]), r=["mf"], w=["mb"])
                rec.dve(lambda e: e.memset(macc[:, 0, :], 0.0), w=["macc"])
                for tk in range(16):
                    def acc(tk=tk):
                        rec.dve(lambda e: e.tensor_tensor(out=macc[:, tk + 1, :], in0=macc[:, tk, :], in1=mf[:, tk, :], op=ALU.add),
                                r=["mf", "macc"], w=["macc"])
                    acc()
                rec.dve(lambda e: e.tensor_copy(out=maccb[:], in_=macc[:]), r=["macc"], w=["maccb"])
                rbank = g.ps[2]
                for tk in range(16):
                    def rk(tk=tk):
                        rec.pe(lambda e: e.matmul(rbank[:, tk * 8:(tk + 1) * 8], lhsT=g.mstrict[:], rhs=mb[:, tk, :], start=True, stop=False),
                               r=["mb", "consts"], w=["rbank"])
                        rec.pe(lambda e: e.matmul(rbank[:, tk * 8:(tk + 1) * 8], lhsT=g.ones_b[:], rhs=maccb[:, tk, :], start=False, stop=True),
                               r=["maccb", "consts"], w=["rbank"])
                    rk()
                rec.dve(lambda e: e.scalar_tensor_tensor(out=rtmp[:], in0=rbank[:, 0:128].rearrange("p (t e) -> p t e", e=NE),
                                                         scalar=1.0, in1=mf[:], op0=ALU.add, op1=ALU.mult),
                        r=["rbank", "mf"], w=["rtmp"])
                rec.dve(lambda e: e.tensor_scalar(out=g.rankm[:], in0=rtmp[:], scalar1=-1.0, scalar2=None, op0=ALU.add),
                        r=["rtmp"], w=["rankm"])
                cbank = g.ps[3]
                rec.pe(lambda e: e.matmul(cbank[:, 0:8], lhsT=g.ones_b[:], rhs=maccb[:, 16, :], start=True, stop=True),
                       r=["maccb", "consts"], w=["cbank"])
                rec.dve(lambda e: e.tensor_reduce(out=cmax[:, 0:1], in_=cbank[:, 0:8], axis=mybir.AxisListType.X, op=ALU.max),
                        r=["cbank"], w=["cmax"])
                rec.dve(lambda e: e.tensor_scalar(out=cmax[:, 1:2], in0=cmax[:, 0:1], scalar1=float(MOE_CAP), scalar2=None,
                                                  op0=ALU.is_gt), r=["cmax"], w=["cmax"])
                rec.dve(lambda e: e.tensor_copy(out=g.flag_i[:, 0:1], in_=cmax[:, 1:2]), r=["cmax"], w=["flag"])
        phase(g, b)


def phase_wo(g, layer):
    nc = g.nc
    from contextlib import ExitStack
    with ExitStack() as es:
        wo = es.enter_context(nc.sbuf_tensor(f"wo_p{g.nphase}", [128, DC, D], BF16))

        def b(rec):
            src = g.w_o[layer].rearrange("(c p) e -> p c e", p=128)
            for h2 in range(2):
                rec.dma("pool", lambda e, h2=h2: e.dma_start(out=wo[:, h2 * 4:(h2 + 1) * 4, :], in_=src[:, h2 * 4:(h2 + 1) * 4, :]),
                        w=[("wo", h2)])
            k = 0
            for tt in range(NTT):
                cs = slice(tt * TT, (tt + 1) * TT)
                for ec in range(DC):
                    bank = g.ps[k % 4]
                    bn = ("ps", k % 4)
                    for c in range(DC):
                        rec.pe(lambda e, c=c, ec=ec, bank=bank, cs=cs: e.matmul(
                            bank[:], lhsT=wo[:, c, ec * 128:(ec + 1) * 128], rhs=g.oT[:, c, cs],
                            start=(c == 0), stop=(c == DC - 1)), r=[("wo", c // 4)], w=[bn])
                    rec.dve(lambda e, ec=ec, bank=bank, cs=cs: e.tensor_tensor(
                        out=g.hT[:, ec, cs], in0=bank[:], in1=g.hT[:, ec, cs], op=ALU.add), r=[bn], w=[("hT", ec, tt)])
                    k += 1
        phase(g, b)


class FfnBufs:
    pass


def alloc_ffn_bufs(g, es, NS=3):
    nc = g.nc
    T = lambda name, shape, dt: es.enter_context(nc.sbuf_tensor(f"{name}_p{g.nphase}", shape, dt))
    B = FfnBufs()
    B.NS = NS
    B.w1g = [T(f"w1g{i}", [128, DC, 256], BF16) for i in range(NS)]
    B.w3g = [T(f"w3g{i}", [128, DC, 256], BF16) for i in range(NS)]
    B.w2g = [T(f"w2g{i}", [128, 2, D], BF16) for i in range(NS)]
    B.hid = [T(f"hid{i}", [128, 2, TT], BF16) for i in range(2)]
    B.sl = [T(f"sl{i}", [128, TT], F32) for i in range(2)]
    B.slg = [T(f"slg{i}", [128, TT], F32) for i in range(2)]
    return B


def emit_w_load(g, rec, B, grp, gidx):
    w1, w3, w2, fg, gi = grp
    s = gidx % B.NS
    fs = slice(fg * 256, (fg + 1) * 256)
    rec.dma("pool", lambda e: e.dma_start(out=B.w1g[s][:], in_=w1.rearrange("(c p) f -> p c f", p=128)[:, :, fs]),
            w=[("w1g", s)])
    rec.dma("pool", lambda e: e.dma_start(out=B.w3g[s][:], in_=w3.rearrange("(c p) f -> p c f", p=128)[:, :, fs]),
            w=[("w3g", s)])
    rec.dma("pool", lambda e: e.dma_start(out=B.w2g[s][:], in_=w2[fs, :].rearrange("(i p) e -> p i e", p=128)),
            w=[("w2g", s)])


def emit_ffn(g, rec, B, experts):
    NS = B.NS
    w1g, w3g, w2g, hid, sl, slg = B.w1g, B.w3g, B.w2g, B.hid, B.sl, B.slg
    psA = [g.ps[0], g.ps[1]]
    psB = [g.ps[2], g.ps[3]]
    psO = [g.ps[4], g.ps[5], g.ps[6], g.ps[7]]
    groups = []
    for (w1, w3, w2, F, gi) in experts:
        for fg in range(F // 256):
            groups.append((w1, w3, w2, fg, gi))

    def load(gidx):
        emit_w_load(g, rec, B, groups[gidx], gidx)

    steps = [(gi_, tt) for gi_ in range(len(groups)) for tt in range(NTT)]
    cnt = {"a": 0, "o": 0, "sl": 0}

    def stage_ab(n):
        gidx, tt = steps[n]
        s = gidx % NS
        gate = groups[gidx][4]
        cs = slice(tt * TT, (tt + 1) * TT)
        hs = n % 2
        for fi in range(2):
            ai = cnt["a"] % 2
            cnt["a"] += 1
            ba, bb = psA[ai], psB[ai]
            for c in range(DC):
                rec.pe(lambda e, c=c, fi=fi, ba=ba: e.matmul(ba[:], lhsT=w1g[s][:, c, fi * 128:(fi + 1) * 128],
                                                             rhs=g.hnT[:, c, cs], start=(c == 0), stop=(c == DC - 1)),
                       r=[("w1g", s)], w=[("psA", ai)])
            for c in range(DC):
                rec.pe(lambda e, c=c, fi=fi, bb=bb: e.matmul(bb[:], lhsT=w3g[s][:, c, fi * 128:(fi + 1) * 128],
                                                             rhs=g.hnT[:, c, cs], start=(c == 0), stop=(c == DC - 1)),
                       r=[("w3g", s)], w=[("psB", ai)])
            si = cnt["sl"] % 2
            cnt["sl"] += 1
            rec.act(lambda e, si=si, ba=ba: e.activation(out=sl[si][:], in_=ba[:], func=AF.Silu),
                    r=[("psA", ai)], w=[("sl", si)])
            if gate is None:
                rec.dve(lambda e, si=si, bb=bb, fi=fi: e.tensor_tensor(out=hid[hs][:, fi, :], in0=bb[:], in1=sl[si][:],
                                                                       op=ALU.mult),
                        r=[("psB", ai), ("sl", si)], w=[("hid", hs, fi)])
            else:
                rec.pool(lambda e, si=si: e.tensor_tensor(out=slg[si][:], in0=sl[si][:], in1=g.oT[:, gate, cs],
                                                          op=ALU.mult),
                         r=[("sl", si), ("G", gate, tt)], w=[("slg", si)])
                rec.dve(lambda e, si=si, bb=bb, fi=fi: e.tensor_tensor(out=hid[hs][:, fi, :], in0=bb[:], in1=slg[si][:],
                                                                       op=ALU.mult),
                        r=[("psB", ai), ("slg", si)], w=[("hid", hs, fi)])

    def stage_out(n):
        gidx, tt = steps[n]
        s = gidx % NS
        cs = slice(tt * TT, (tt + 1) * TT)
        hs = n % 2
        for ec in range(DC):
            oi = cnt["o"] % 4
            cnt["o"] += 1
            bo = psO[oi]
            for fi in range(2):
                rec.pe(lambda e, fi=fi, ec=ec, bo=bo: e.matmul(bo[:], lhsT=w2g[s][:, fi, ec * 128:(ec + 1) * 128],
                                                               rhs=hid[hs][:, fi, :], start=(fi == 0), stop=(fi == 1)),
                       r=[("w2g", s), ("hid", hs, fi)], w=[("psO", oi)])
            rec.dve(lambda e, ec=ec, bo=bo: e.tensor_tensor(out=g.hT[:, ec, cs], in0=bo[:], in1=g.hT[:, ec, cs],
                                                            op=ALU.add),
                    r=[("psO", oi)], w=[("hT", ec, tt)])

    load(0)
    if len(groups) > 1 and NS > 2:
        load(1)
    for n in range(len(steps) + 1):
        if n < len(steps):
            gidx, tt = steps[n]
            if NS > 2:
                if tt == 1 and gidx + 2 < len(groups):
                    load(gidx + 2)
            else:
                if tt == 1 and gidx + 1 < len(groups):
                    load(gidx + 1)
            stage_ab(n)
        if n >= 1:
            stage_out(n - 1)


def phase_ffn(g, experts):
    from contextlib import ExitStack
    with ExitStack() as es:
        B = alloc_ffn_bufs(g, es, 3)
        phase(g, lambda rec: emit_ffn(g, rec, B, experts))


def phase_ffn_dense(g):
    phase_norm(g, 1)
    phase_ffn(g, [(g.w1, g.w3, g.w2, DFF, None)])


def phase_moe(g):
    phase_norm(g, 3, router=True)
    phase_moe_experts(g)


def emit_gbcast(g, rec, diag):
    G = g.oT
    gt = g.gt
    k = 0
    for ex in range(NE):
        for tt in range(NTT):
            def one(ex=ex, tt=tt, k=k):
                bank = g.ps[2 + k % 2]
                bn = ("psA", k % 2) if False else ("psB", k % 2)
                for j in range(4):
                    def sub(j=j):
                        tk = tt * 4 + j
                        dg = diag[(k * 4 + j) % len(diag)]
                        dn = ("diag", (k * 4 + j) % len(diag))
                        rec.dve(lambda e: e.tensor_scalar(out=dg[:], in0=g.ident_f[:], scalar1=gt[:, tk, ex:ex + 1],
                                                          scalar2=None, op0=ALU.mult), r=["consts"], w=[dn])
                        rec.pe(lambda e: e.matmul(bank[:, j * 128:(j + 1) * 128], lhsT=g.ones_f[:], rhs=dg[:], start=True, stop=True),
                               r=[dn, "consts"], w=[bn])
                    sub()
                rec.act(lambda e: e.activation(out=G[:, ex, tt * TT:(tt + 1) * TT], in_=bank[:], func=AF.Copy),
                        r=[bn], w=[("G", ex, tt)])
            one()
            k += 1


def emit_moe_sparse(g, rec, B, X):
    C = MOE_CAP
    NJ = C // 128
    CTS = [(0, 384), (384, C - 384)] if C > 384 else [(0, C)]
    hn_tok = g.oT.rearrange("p c (a b) -> p (c a) b", b=1024)
    hn_tok = g.oT[:].rearrange("p c s -> p (c s)").rearrange("p (t d) -> p t d", d=D)
    hnflat = g.hnT[:].rearrange("p c s -> p (c s)")
    Sel = hnflat[:, 0:16 * C].rearrange("p (t j) -> p t j", j=C)
    Xe = hnflat[:, 16 * C:16 * C + DC * C].rearrange("p (c j) -> p c j", j=C)
    Ybf = hnflat[:, 0:NJ * D].rearrange("p (j d) -> p j d", d=D)
    psbf = [g.ps[i][:].bitcast(BF16) for i in range(8)]
    w1g, w3g, w2g, hid, sl = B.w1g, B.w3g, B.w2g, B.hid, B.sl
    NS = B.NS
    rec.pool(lambda e: e.iota(X.iota_i, pattern=[[1, C]], base=0, channel_multiplier=0), w=["iota_i"])
    rec.dve(lambda e: e.tensor_copy(out=X.iotaC[:], in_=X.iota_i), r=["iota_i"], w=["iotaC"])
    rec.pool(lambda e: e.iota(X.jcol_i[:], pattern=[[128, NJ]], base=0, channel_multiplier=1), w=["jcol_i"])
    rec.dve(lambda e: e.tensor_copy(out=X.jcol[:], in_=X.jcol_i[:]), r=["jcol_i"], w=["jcol"])
    for tk in range(16):
        def tr(tk=tk):
            bi = tk % 2
            for c in range(DC):
                def t1(c=c):
                    rec.pe(lambda e: e.transpose(psbf[bi][:, c * 128:(c + 1) * 128], g.hnT[:, c, tk * 128:(tk + 1) * 128], g.ident_b[:]),
                           r=[("hnT", c, tk // 4), "consts"], w=[("psA", bi)])
                t1()
            if tk % 2 == 0:
                rec.act(lambda e: e.activation(out=hn_tok[:, tk, :], in_=psbf[bi][:, :], func=AF.Copy),
                        r=[("psA", bi)], w=[("hn_tok", tk)])
            else:
                rec.dve(lambda e: e.tensor_copy(out=hn_tok[:, tk, :], in_=psbf[bi][:, :]),
                        r=[("psA", bi)], w=[("hn_tok", tk)])
        tr()
    all_hn_tok = [("hn_tok", tk) for tk in range(16)]
    groups_all = []
    for ex in range(NE):
        for fg in range(DFE // 256):
            groups_all.append((g.e_w1[ex], g.e_w3[ex], g.e_w2[ex], fg, ex))
    NG = DFE // 256
    emit_w_load(g, rec, B, groups_all[0], 0)
    cnt = {"k": 0}

    def rot(name, n):
        v = cnt.get(name, 0)
        cnt[name] = v + 1
        return v % n

    def sel_ybf_overlap(tk, jj):
        return tk * C < (jj + 1) * D and jj * D < (tk + 1) * C

    def build_sel(ex, tks):
        for tk in tks:
            def mk(tk=tk):
                rec.dve(lambda e: e.tensor_scalar(out=Sel[:, tk, :], in0=X.iotaC[:], scalar1=g.rankm[:, tk, ex:ex + 1],
                                                  scalar2=None, op0=ALU.is_equal),
                        r=["iotaC"] + (all_hn_tok if ex == 0 else []),
                         w=[("Sel", tk)] + [("Ybf", jj) for jj in range(NJ) if sel_ybf_overlap(tk, jj)])
            mk()

    build_sel(0, range(16))
    for ex in range(NE):
        def expert(ex=ex):
            for c in range(DC):
                for (c0, cw) in CTS:
                    def ga(c=c, c0=c0, cw=cw):
                        bi = rot("psg", 2)
                        bank = g.ps[bi]
                        for tk in range(16):
                            def mm(tk=tk):
                                rec.pe(lambda e: e.matmul(bank[:, 0:cw], lhsT=hn_tok[:, tk, c * 128:(c + 1) * 128],
                                                          rhs=Sel[:, tk, c0:c0 + cw], start=(tk == 0), stop=(tk == 15)),
                                       r=[("hn_tok", tk), ("Sel", tk)], w=[("psA", bi)])
                            mm()
                        if (c + (c0 > 0)) % 2 == 0:
                            rec.act(lambda e: e.activation(out=Xe[:, c, c0:c0 + cw], in_=bank[:, 0:cw], func=AF.Copy),
                                    r=[("psA", bi)], w=[("Xe", c, c0)])
                        else:
                            rec.dve(lambda e: e.tensor_copy(out=Xe[:, c, c0:c0 + cw], in_=bank[:, 0:cw]),
                                    r=[("psA", bi)], w=[("Xe", c, c0)])
                    ga()
            gb = g.ps[2]
            for jt in range(NJ):
                for tk in range(16):
                    def gm_(jt=jt, tk=tk):
                        rec.pe(lambda e: e.matmul(gb[:, jt:jt + 1], lhsT=Sel[:, tk, jt * 128:(jt + 1) * 128],
                                                  rhs=g.gtb[:, tk, ex:ex + 1], start=(tk == 0), stop=(tk == 15)),
                               r=[("Sel", tk)], w=[("psB", 0)])
                    gm_()
            rec.dve(lambda e: e.tensor_copy(out=X.gcomp[:], in_=gb[:, 0:NJ]), r=[("psB", 0)], w=["gcomp"])
            prep_state = {}

            def prep(tt):
                ri = rot("rb", 2)
                rbank = g.ps[ri]
                for j4 in range(4):
                    def bc(j4=j4):
                        tk = tt * 4 + j4
                        di = rot("diag", 3)
                        rec.dve(lambda e: e.tensor_scalar(out=X.diag[di][:], in0=g.ident_f[:], scalar1=g.rankm[:, tk, ex:ex + 1],
                                                          scalar2=None, op0=ALU.mult), r=["consts"], w=[("diag", di)])
                        rec.pe(lambda e: e.matmul(rbank[:, j4 * 128:(j4 + 1) * 128], lhsT=g.ones_f[:], rhs=X.diag[di][:],
                                                  start=True, stop=True), r=[("diag", di), "consts"], w=[("psA", ri)])
                    bc()
                rbi = rot("RB", 1)
                rec.act(lambda e: e.activation(out=X.RB[rbi][:], in_=rbank[:], func=AF.Copy), r=[("psA", ri)], w=[("RB", rbi)])
                sti = rot("SelT", 2)
                SelT = X.SelT[sti]
                for jt in range(NJ):
                    def st(jt=jt):
                        rec.dve(lambda e: e.tensor_scalar(out=SelT[:, jt, :], in0=X.RB[rbi][:], scalar1=X.jcol[:, jt:jt + 1],
                                                          scalar2=None, op0=ALU.is_equal),
                                r=[("RB", rbi), "jcol"], w=[("SelT", sti, jt)])
                    st()
                prep_state[tt] = (sti, SelT)

            def yb(jt):
                rec.act(lambda e: e.activation(out=Ybf[:, jt, :], in_=X.Yacc[:, jt, :], func=AF.Copy, scale=X.gcomp[:, jt:jt + 1]),
                        r=[("Yacc", jt, 0), ("Yacc", jt, 1), "gcomp"],
                        w=[("Ybf", jt)] + [("Sel", tk) for tk in range(16) if sel_ybf_overlap(tk, jt)])

            for fg in range(NG):
                def fgroup(fg=fg):
                    gidx = ex * NG + fg
                    s = gidx % NS
                    if gidx + 1 < len(groups_all):
                        emit_w_load(g, rec, B, groups_all[gidx + 1], gidx + 1)
                    if fg == NG - 1:
                        prep(0)
                    if fg == NG - 5 and ex + 1 < NE:
                        build_sel(ex + 1, [tk for tk in range(16) if not any(sel_ybf_overlap(tk, jj) for jj in range(NJ))])
                    for (c0, cw) in CTS:
                        def abpart(c0=c0, cw=cw):
                            hs = rot("hid", 2)
                            for fi in range(2):
                                def ab(fi=fi):
                                    ai = rot("ab", 2)
                                    ba, bb = g.ps[ai], g.ps[2 + ai]
                                    for c in range(DC):
                                        def m1(c=c):
                                            rec.pe(lambda e: e.matmul(ba[:, 0:cw], lhsT=w1g[s][:, c, fi * 128:(fi + 1) * 128],
                                                                      rhs=Xe[:, c, c0:c0 + cw], start=(c == 0), stop=(c == DC - 1)),
                                                   r=[("w1g", s), ("Xe", c, c0)], w=[("psA", ai)])
                                        m1()
                                    for c in range(DC):
                                        def m3(c=c):
                                            rec.pe(lambda e: e.matmul(bb[:, 0:cw], lhsT=w3g[s][:, c, fi * 128:(fi + 1) * 128],
                                                                      rhs=Xe[:, c, c0:c0 + cw], start=(c == 0), stop=(c == DC - 1)),
                                                   r=[("w3g", s), ("Xe", c, c0)], w=[("psB", ai)])
                                        m3()
                                    si = rot("sl", 2)
                                    rec.act(lambda e: e.activation(out=sl[si][:, 0:cw], in_=ba[:, 0:cw], func=AF.Silu),
                                            r=[("psA", ai)], w=[("sl", si)])
                                    rec.dve(lambda e: e.tensor_tensor(out=hid[hs][:, fi, 0:cw], in0=bb[:, 0:cw], in1=sl[si][:, 0:cw],
                                                                      op=ALU.mult),
                                            r=[("psB", ai), ("sl", si)], w=[("hid", hs, fi)])
                                ab()
                            for jl in range(cw // 128):
                                jt = c0 // 128 + jl
                                for dh in range(2):
                                    def outp(jl=jl, jt=jt, dh=dh):
                                        oi = rot("o", 4)
                                        bo = g.ps[4 + oi]
                                        for fi in range(2):
                                            def mo(fi=fi):
                                                rec.pe(lambda e: e.matmul(bo[:], lhsT=hid[hs][:, fi, jl * 128:(jl + 1) * 128],
                                                                          rhs=w2g[s][:, fi, dh * 512:(dh + 1) * 512],
                                                                          start=(fi == 0), stop=(fi == 1)),
                                                       r=[("w2g", s), ("hid", hs, fi)], w=[("psO", oi)])
                                            mo()
                                        if fg == 0:
                                            rec.dve(lambda e: e.tensor_copy(out=X.Yacc[:, jt, dh * 512:(dh + 1) * 512], in_=bo[:]),
                                                    r=[("psO", oi)], w=[("Yacc", jt, dh)])
                                        else:
                                            rec.dve(lambda e: e.tensor_tensor(out=X.Yacc[:, jt, dh * 512:(dh + 1) * 512], in0=bo[:],
                                                                              in1=X.Yacc[:, jt, dh * 512:(dh + 1) * 512], op=ALU.add),
                                                    r=[("psO", oi)], w=[("Yacc", jt, dh)])
                                    outp()
                                if fg == NG - 1:
                                    yb(jt)
                        abpart()
                fgroup()
            for tt in range(NTT):
                def sc(tt=tt):
                    sti, SelT = prep_state[tt]
                    for dc in range(DC):
                        def so(dc=dc):
                            oi = rot("o", 4)
                            bo = g.ps[4 + oi]
                            for jt in range(NJ):
                                def mm(jt=jt):
                                    rec.pe(lambda e: e.matmul(bo[:], lhsT=Ybf[:, jt, dc * 128:(dc + 1) * 128], rhs=SelT[:, jt, :],
                                                              start=(jt == 0), stop=(jt == NJ - 1)),
                                           r=[("Ybf", jt), ("SelT", sti, jt)], w=[("psO", oi)])
                                mm()
                            rec.dve(lambda e: e.tensor_tensor(out=g.hT[:, dc, tt * TT:(tt + 1) * TT], in0=bo[:],
                                                              in1=g.hT[:, dc, tt * TT:(tt + 1) * TT], op=ALU.add),
                                    r=[("psO", oi)], w=[("hT", dc, tt)])
                            for _ in range(FILL_MOE):
                                rec.pe(lambda e: e.matmul(g.ps[3][:], lhsT=g.ident_b[:], rhs=hn_tok[:, 0, 0:TT], start=True, stop=True),
                                       r=["consts"], w=[("psB", 1)])
                        so()
                        if dc == 3 and tt + 1 < NTT:
                            prep(tt + 1)
                sc()
            if ex + 1 < NE:
                build_sel(ex + 1, [tk for tk in range(16) if any(sel_ybf_overlap(tk, jj) for jj in range(NJ))])
        expert()


class Rec2:
    pass


def phase_moe_experts(g):
    nc = g.nc
    from contextlib import ExitStack
    k = g.nphase
    g.nphase += 1
    with ExitStack() as es:
        g.nphase -= 1
        B = alloc_ffn_bufs(g, es, 2)
        T = lambda name, shape, dt: es.enter_context(nc.sbuf_tensor(f"{name}_p{k}", shape, dt))
        X = Ctx()
        C = MOE_CAP
        NJ = C // 128
        X.iotaC = T("iotaC", [128, C], F32)
        X.jcol_i = T("jcol_i", [128, NJ], mybir.dt.int32)
        X.jcol = T("jcol", [128, NJ], F32)
        X.gcomp = T("gcomp", [128, NJ], F32)
        X.Yacc = T("Yacc", [128, NJ, D], F32)
        X.iota_i = X.Yacc[:, 0, 0:C].bitcast(mybir.dt.int32)
        X.RB = [T(f"RB{i}", [128, TT], F32) for i in range(1)]
        X.SelT = [T(f"SelT{i}", [128, NJ, TT], BF16) for i in range(2)]
        X.diag = [T(f"diag{i}", [128, 128], F32) for i in range(3)]
        g.nphase += 1

        recD = Rec(nc, True)
        emit_gbcast(g, recD, X.diag)
        emit_ffn(g, recD, B, [(g.e_w1[ex], g.e_w3[ex], g.e_w2[ex], DFE, ex) for ex in range(NE)])
        recS = Rec(nc, True)
        emit_moe_sparse(g, recS, B, X)

        cnt0, dcnt0 = dict(g.cnt), dict(g.dcnt)
        cntD, dcntD = dict(cnt0), dict(dcnt0)
        cntS, dcntS = dict(cnt0), dict(dcnt0)
        perD = recD.emit(g.sems, g.dma_sems, cntD, dcntD)
        perS = recS.emit(g.sems, g.dma_sems, cntS, dcntS)
        for e in ENGS:
            g.cnt[e] = max(cntD[e], cntS[e])
            g.dcnt[e] = max(dcntD[e], dcntS[e])

        def dma_final(rec):
            final = {}
            for op in rec.ops:
                if op.dma:
                    final[(op.eng, id(op.sem))] = (op.sem, op.val)
            return final

        def dma_sem_val(q, j, n):
            kk = len(g.dma_sems[q])
            if kk == 0:
                return 0
            return 16 * ((n - j + kk - 1) // kk) if n > j else 0

        def branch(eng_name, eng, rec, per, cntX, dcntX):
            rec.run_engine(eng_name, eng, per)
            for (q, _), (s_, v) in dma_final(rec).items():
                if q == eng_name:
                    eng.wait_ge(s_, v)

        with nc.Block() as block:
            def mk(eng_name):
                def body(eng):
                    eng.wait_ge(g.bar, 5 * k)
                    reg = eng.alloc_register(f"ovf_{eng_name}")
                    eng.reg_load(reg, g.flag_i[0:1, 0:1])
                    with eng.If(eng.snap(reg) > 0):
                        branch(eng_name, eng, recD, perD, cntD, dcntD)
                    with eng.Else():
                        branch(eng_name, eng, recS, perS, cntS, dcntS)
                    eng.drain().then_inc(g.bar, 1)
                return body
            block.tensor(mk("pe"))
            block.vector(mk("dve"))
            block.scalar(mk("act"))
            block.gpsimd(mk("pool"))
            block.sync(mk("sp"))
        g.sems, g.dma_sems = g.sems2, g.dma_sems2
        g.cnt = {e: 0 for e in ENGS}
        g.dcnt = {e: 0 for e in ENGS}


def phase_final(g):
    nc = g.nc
    from contextlib import ExitStack
    with ExitStack() as es:
        T = lambda name, shape, dt: es.enter_context(nc.sbuf_tensor(f"{name}_p{g.nphase}", shape, dt))
        sq = [T(f"sq{i}", [128, TT], BF16) for i in range(4)]
        lnv = [T(f"lnv{i}", [128, TT], F32) for i in range(2)]
        rstd = [T(f"rstd{i}", [128, TT], F32) for i in range(2)]
        yT = [T(f"yT{i}", [128, TT], F32) for i in range(3)]
        ystage = [T(f"ystage{i}", [128, 4, D], F32) for i in range(2)]

        def b(rec):
            yv = g.y.rearrange("(t j p) d -> t p j d", j=4, p=128)
            cnt = {"k": 0}

            def out_f32(rec, tt, c, gcol, r_, rs_n):
                cs = slice(tt * TT, (tt + 1) * TT)
                k = cnt["k"]
                cnt["k"] += 1
                yt = yT[k % 3]
                yn = ("yT", k % 3)
                bank = g.ps[2 + k % 4]
                bn = ("pst", k % 4)
                st = ystage[tt % 2]
                rec.dve(lambda e: e.scalar_tensor_tensor(out=yt[:], in0=g.hT[:, c, cs], scalar=gcol, in1=r_[:],
                                                         op0=ALU.mult, op1=ALU.mult),
                        r=[("hT", c, tt), rs_n, "consts"], w=[yn])
                for j in range(4):
                    rec.pe(lambda e, j=j: e.transpose(bank[:, j * 128:(j + 1) * 128], yt[:, j * 128:(j + 1) * 128], g.ident_f[:]),
                           r=[yn], w=[bn])
                rec.act(lambda e: e.activation(out=st[:, :, c * 128:(c + 1) * 128],
                                               in_=bank[:].rearrange("p (j d) -> p j d", j=4), func=AF.Copy),
                        r=[bn], w=[("ystage", tt % 2, c)])
                if c == DC - 1:
                    rec.dma("sp", lambda e: e.dma_start(out=yv[tt], in_=st[:]),
                            r=[("ystage", tt % 2, cc) for cc in range(DC)], w=[("ystage_out", tt % 2)])

            emit_rmsnorm(g, rec, 4, sq, lnv, rstd, [g.ps[0], g.ps[1]], out_bf=False, out_f32=out_f32)
        phase(g, b)


def phase_attn(g, layer):
    phase_norm(g, 2 * layer)
    dbg_o = getattr(g, "stop", None) == f"attn{layer}_o"
    if layer == 0:
        phase_attn_sb(g, fuse_wo=not dbg_o)
    else:
        phase_attn_moba(g, fuse_wo=not dbg_o)
    if dbg_o:
        def b(rec):
            for c in range(DC):
                def one(c=c):
                    rec.dve(lambda e: e.tensor_copy(out=g.hT[:, c, :], in_=g.oT[:, c, :]), w=[("hT", c)])
                one()
        phase(g, b)
        return


def _emit_qkv_weights(g, rec, layer, hp, wq, wk, wv):
    s = hp % 2
    wsrc = g.w_qkv[layer].rearrange("(c p) n -> p c n", p=128)
    for i, (wt, nm) in enumerate(((wq, "wq"), (wk, "wk"), (wv, "wv"))):
        def one(wt=wt, nm=nm, i=i):
            cols = slice(i * D + hp * 128, i * D + (hp + 1) * 128)
            rec.dma("pool", lambda e: e.dma_start(out=wt[s][:], in_=wsrc[:, :, cols]), w=[(nm, s)])
        one()


def _emit_fill(g, rec, psP, rotP, n):
    for _ in range(n):
        pi = rotP.next()
        bank = psP[pi]
        rec.pe(lambda e, bank=bank: e.matmul(bank[:], lhsT=g.ident_b[:], rhs=g.hnT[:, 0, 0:TT], start=True, stop=True),
               r=["consts"], w=[("psP", pi)])


def _emit_wo_load(g, rec, wo, layer):
    src = g.w_o[layer].rearrange("(c p) e -> p c e", p=128)
    for h2 in range(2):
        rec.dma("pool", lambda e, h2=h2: e.dma_start(out=wo[:, h2 * 4:(h2 + 1) * 4, :], in_=src[:, h2 * 4:(h2 + 1) * 4, :]),
                w=[("wo", h2)])


def _emit_wo(g, rec, wo, psP, rotP):
    for tt in range(NTT):
        for ec in range(DC):
            def grp(tt=tt, ec=ec):
                cs = slice(tt * TT, (tt + 1) * TT)
                pi = rotP.next()
                bank = psP[pi]
                for c in range(DC):
                    rec.pe(lambda e, c=c: e.matmul(bank[:], lhsT=wo[:, c, ec * 128:(ec + 1) * 128], rhs=g.oT[:, c, cs],
                                                   start=(c == 0), stop=(c == DC - 1)),
                           r=[("wo", c // 4), ("oT", c, tt, 0), ("oT", c, tt, 1)], w=[("psP", pi)])
                rec.dve(lambda e: e.tensor_tensor(out=g.hT[:, ec, cs], in0=bank[:], in1=g.hT[:, ec, cs], op=ALU.add),
                        r=[("psP", pi)], w=[("hT", ec, tt)])
            grp()


def _emit_v_proj(g, rec, hp, wv, V, psP, rotP):
    s = hp % 2
    for t4 in range(4):
        def one(t4=t4):
            pi = rotP.next()
            bank = psP[pi]
            for j in range(4):
                tok = t4 * 4 + j
                for c in range(DC):
                    def mm(j=j, tok=tok, c=c):
                        rec.pe(lambda e: e.matmul(bank[:, j * 128:(j + 1) * 128], lhsT=g.hnT[:, c, tok * 128:(tok + 1) * 128],
                                                  rhs=wv[s][:, c, :], start=(c == 0), stop=(c == DC - 1)),
                               r=[("wv", s), ("hnT", c, tok // 4)], w=[("psP", pi)])
                    mm()
            rec.dve(lambda e: e.tensor_copy(out=V[s][:, t4 * 4:(t4 + 1) * 4, :],
                                            in_=bank[:].rearrange("p (j n) -> p j n", j=4)),
                    r=[("psP", pi)], w=[("V", s, t4)])
        one()


def phase_attn_sb(g, fuse_wo=True):
    nc = g.nc
    from contextlib import ExitStack
    with ExitStack() as es:
        T = lambda name, shape, dt: es.enter_context(nc.sbuf_tensor(f"{name}_p{g.nphase}", shape, dt))
        wq = [T(f"wq{i}", [128, DC, 128], BF16) for i in range(2)]
        wk = [T(f"wk{i}", [128, DC, 128], BF16) for i in range(2)]
        wv = [T(f"wv{i}", [128, DC, 128], BF16) for i in range(2)]
        qm = [[T(f"qm{i}_{hh}", [128, S], BF16) for hh in range(2)] for i in range(2)]
        wo = T("wo", [128, DC, D], BF16) if fuse_wo else None
        kT = [T(f"kT{i}", [128, S], BF16) for i in range(2)]
        V = [T(f"V{i}", [128, 16, 128], BF16) for i in range(2)]
        NB = 3
        E = [T(f"E{i}", [128, TT], F32) for i in range(NB)]
        Lp = [T(f"Lp{i}", [128, TT], BF16) for i in range(NB)]
        Acc = [T(f"Acc{i}", [128, TT], BF16) for i in range(NB)]
        W = [T(f"W{i}", [128, TT], BF16) for i in range(NB)]
        psZ = [g.ps[0], g.ps[1]]
        psC = [g.ps[2], g.ps[3]]
        psO = [g.ps[4], g.ps[5]]
        psP = [g.ps[6], g.ps[7]]

        def b(rec):
            rotZ, rotC, rotO, rotP = Rot(2), Rot(2), Rot(2), Rot(2)
            rotE, rotL, rotA, rotW = Rot(NB), Rot(NB), Rot(NB), Rot(NB)

            def proj_ops(hp):
                ops = []
                s = hp % 2
                for tt in range(NTT):
                    for kind in ("q", "k"):
                        st = {}
                        for c in range(DC):
                            def mm(c=c, tt=tt, kind=kind, st=st):
                                if c == 0:
                                    st["pi"] = rotP.next()
                                pi = st["pi"]
                                bank = psP[pi]
                                wt, wn = (wq, "wq") if kind == "q" else (wk, "wk")
                                cs = slice(tt * TT, (tt + 1) * TT)
                                rec.pe(lambda e: e.matmul(bank[:], lhsT=wt[s][:, c, :], rhs=g.hnT[:, c, cs],
                                                          start=(c == 0), stop=(c == DC - 1)),
                                       r=[(wn, s), ("hnT", c, tt)], w=[("psP", pi)])
                                if c == DC - 1:
                                    if kind == "q":
                                        for hh in range(2):
                                            hb = hh * 64
                                            rec.dve(lambda e, hh=hh, hb=hb: e.tensor_scalar(
                                                out=qm[s][hh][hb:hb + 64, cs], in0=bank[hb:hb + 64, :], scalar1=DH ** -0.5,
                                                scalar2=None, op0=ALU.mult), r=[("psP", pi), ("qz", s, hh)], w=[("q", s, tt, hh)])
                                    else:
                                        rec.dve(lambda e: e.tensor_copy(out=kT[s][:, cs], in_=bank[:]),
                                                r=[("psP", pi)], w=[("k", s, tt)])
                            ops.append(mm)
                for t4 in range(4):
                    st = {}
                    for j in range(4):
                        for ch in range(2):
                            def vm(t4=t4, j=j, ch=ch, st=st):
                                if j == 0 and ch == 0:
                                    st["pi"] = rotP.next()
                                pi = st["pi"]
                                bank = psP[pi]
                                tok = t4 * 4 + j
                                for c in range(ch * 4, ch * 4 + 4):
                                    rec.pe(lambda e, c=c: e.matmul(bank[:, j * 128:(j + 1) * 128],
                                                                   lhsT=g.hnT[:, c, tok * 128:(tok + 1) * 128], rhs=wv[s][:, c, :],
                                                                   start=(c == 0), stop=(c == DC - 1)),
                                           r=[("wv", s), ("hnT", c, tok // 4)], w=[("psP", pi)])
                                if j == 3 and ch == 1:
                                    rec.dve(lambda e: e.tensor_copy(out=V[s][:, t4 * 4:(t4 + 1) * 4, :],
                                                                    in_=bank[:].rearrange("p (j n) -> p j n", j=4)),
                                            r=[("psP", pi)], w=[("V", s, t4)])
                            ops.append(vm)
                return ops

            def make_tiles(hp):
                tiles = []
                for hh in range(2):
                    for tt in range(NTT):
                        chain = []
                        for i, kb in enumerate(range(4 * tt + 3, -1, -1)):
                            j = kb - 4 * tt
                            t = Ctx()
                            t.hp, t.hh, t.tt, t.kb, t.i = hp, hh, tt, kb, i
                            t.c0 = 128 * j if j >= 0 else 0
                            t.diag = j >= 0
                            t.last = kb == 0
                            t.ain = None
                            chain.append(t)
                        for a, bb in zip(chain[:-1], chain[1:]):
                            a.nxt = bb
                        chain[-1].nxt = None
                        tiles += chain
                return tiles

            def qk_aps(t):
                s = t.hp % 2
                hb = t.hh * 64
                lhsT = kT[s][:, t.kb * 128:(t.kb + 1) * 128]
                rhs = qm[s][t.hh][:, t.tt * TT + t.c0:(t.tt + 1) * TT]
                deps = [("k", s, t.kb // 4), ("q", s, t.tt, t.hh)]
                return lhsT, rhs, deps

            def s1(t):
                c0 = t.c0
                lhsT, rhs, deps = qk_aps(t)
                zb = rotZ.next()
                rec.pe(lambda e: e.matmul(psZ[zb][:, c0:], lhsT=lhsT, rhs=rhs, start=True, stop=True),
                       r=deps, w=[("Z", zb)])
                eb = rotE.next()
                rec.act(lambda e: e.activation(out=E[eb][:, c0:], in_=psZ[zb][:, c0:], func=AF.Exp),
                        r=[("Z", zb)], w=[("E", eb)])
                lb = rotL.next()
                t.lb = lb
                rec.act(lambda e: e.activation(out=Lp[lb][:, c0:], in_=E[eb][:, c0:], func=AF.Ln, bias=1.0),
                        r=[("E", eb)], w=[("Lp", lb)])
                if t.diag:
                    rec.dve(lambda e: e.tensor_tensor(out=Lp[lb][:, c0:c0 + 128], in0=Lp[lb][:, c0:c0 + 128],
                                                      in1=g.mstrict[:], op=ALU.mult),
                            r=[("Lp", lb), "consts"], w=[("Lp", lb)])
                if t.nxt is not None:
                    ab = rotA.next()
                    t.nxt.ain = ab
                    c0n = t.nxt.c0
                    if c0n < c0:
                        rec.pool(lambda e: e.memset(Acc[ab][:, c0n:c0], 0.0), w=[("Acc", ab)])
                    if t.i == 0:
                        rec.pool(lambda e: e.tensor_copy(out=Acc[ab][:, c0:], in_=Lp[lb][:, c0:]),
                                 r=[("Lp", lb)], w=[("Acc", ab)])
                    else:
                        ain = t.ain
                        rec.pool(lambda e: e.tensor_tensor(out=Acc[ab][:, c0:], in0=Acc[ain][:, c0:], in1=Lp[lb][:, c0:],
                                                           op=ALU.add),
                                 r=[("Lp", lb), ("Acc", ain)], w=[("Acc", ab)])

            def s2(t):
                c0 = t.c0
                lb = t.lb
                lhsT, rhs, deps = qk_aps(t)
                cb = rotC.next()
                rec.pe(lambda e: e.matmul(psC[cb][:, c0:], lhsT=g.neguincl[:], rhs=Lp[lb][:, c0:], start=True, stop=False),
                       r=[("Lp", lb), "consts"], w=[("C", cb)])
                if t.i > 0:
                    ain = t.ain
                    rec.pe(lambda e: e.matmul(psC[cb][:, c0:], lhsT=g.negones_b[:], rhs=Acc[ain][:, c0:], start=False, stop=False),
                           r=[("Acc", ain), "consts"], w=[("C", cb)])
                rec.pe(lambda e: e.matmul(psC[cb][:, c0:], lhsT=lhsT, rhs=rhs, start=False, stop=True),
                       r=deps, w=[("C", cb)])
                wb = rotW.next()
                t.wb = wb
                rec.act(lambda e: e.activation(out=W[wb][:, c0:], in_=psC[cb][:, c0:], func=AF.Exp),
                        r=[("C", cb)], w=[("W", wb)])
                if t.diag:
                    rec.dve(lambda e: e.tensor_tensor(out=W[wb][:, c0:c0 + 128], in0=W[wb][:, c0:c0 + 128],
                                                      in1=g.mstrict[:], op=ALU.mult),
                            r=[("W", wb), "consts"], w=[("W", wb)])

            ostate = {}

            def s3(t):
                c0 = t.c0
                s = t.hp % 2
                hb = t.hh * 64
                M = 128
                if t.i == 0:
                    ob = rotO.next()
                    ostate["ob"] = ob
                    rec.pe(lambda e: e.matmul(psO[ob][0:M, :], lhsT=g.zeros_b[:, 0:M], rhs=g.ones_b[:, 0:1].to_broadcast([128, TT]) if False else g.hnT[:, 0, 0:TT],
                                              start=True, stop=False),
                           r=["consts"], w=[("O", ob)])
                ob = ostate["ob"]
                wb = t.wb
                kb = t.kb
                last = t.last
                rec.pe(lambda e: e.matmul(psO[ob][0:M, c0:], lhsT=V[s][:, kb, 0:M], rhs=W[wb][:, c0:], start=False, stop=last),
                       r=[("V", s, kb // 4), ("W", wb)], w=[("O", ob)])
                if last:
                    hp, tt = t.hp, t.tt
                    rec.dve(lambda e: e.tensor_copy(out=g.oT[hb:hb + 64, hp, tt * TT:(tt + 1) * TT], in_=psO[ob][hb:hb + 64, :]),
                            r=[("O", ob)], w=[("oT", hp, tt, t.hh)])

            _emit_qkv_weights(g, rec, 0, 0, wq, wk, wv)
            for i in range(2):
                def zq(i=i):
                    rec.pool(lambda e: e.memset(qm[i][0][64:128, :], 0.0), w=[("qz", i, 0)])
                    rec.pool(lambda e: e.memset(qm[i][1][0:64, :], 0.0), w=[("qz", i, 1)])
                zq()
            for op in proj_ops(0):
                op()
            for hp in range(DC):
                pending = []
                if hp + 1 < DC:
                    _emit_qkv_weights(g, rec, 0, hp + 1, wq, wk, wv)
                    pending = proj_ops(hp + 1)
                elif fuse_wo:
                    _emit_wo_load(g, rec, wo, 0)
                tiles = make_tiles(hp)
                n = len(tiles)
                for i in range(n + 2):
                    if i < n:
                        s1(tiles[i])
                    if 1 <= i <= n:
                        s2(tiles[i - 1])
                    if i >= 2:
                        s3(tiles[i - 2])
                    for _ in range(FILL_SB):
                        if pending and i >= FILL_DELAY:
                            pending.pop(0)()
                        else:
                            _emit_fill(g, rec, psP, rotP, 1)
                while pending:
                    pending.pop(0)()
            if fuse_wo:
                _emit_wo(g, rec, wo, psP, rotP)
        phase(g, b)


def phase_attn_moba(g, fuse_wo=True):
    nc = g.nc
    from contextlib import ExitStack
    with ExitStack() as es:
        T = lambda name, shape, dt: es.enter_context(nc.sbuf_tensor(f"{name}_p{g.nphase}", shape, dt))
        wq = [T(f"wq{i}", [128, DC, 128], BF16) for i in range(2)]
        wk = [T(f"wk{i}", [128, DC, 128], BF16) for i in range(2)]
        wv = [T(f"wv{i}", [128, DC, 128], BF16) for i in range(2)]
        wo = T("wo", [128, DC, D], BF16) if fuse_wo else None
        qa = [T(f"qa{i}", [128, S], BF16) for i in range(2)]
        ka = [T(f"ka{i}", [128, S], BF16) for i in range(2)]
        V = [T(f"V{i}", [128, 16, 128], BF16) for i in range(2)]
        NB = 4
        P = [T(f"P{i}", [128, TT], BF16) for i in range(NB)]
        lnd = [T(f"lnd{i}", [128, TT], F32) for i in range(2)]
        rden = [T(f"rden{i}", [128, TT], F32) for i in range(2)]
        R = T("R", [128, NH, 2, 128], BF16)
        bstage = [T(f"bstage{i}", [128, 2, 128], F32) for i in range(2)]
        b31 = T("b31", [128, NH], F32)
        nb31 = T("nb31", [128, NH], F32)
        ksum = [T(f"ksum{i}", [64, 8], F32) for i in range(2)]
        kmT = [T(f"kmT{i}", [64, 8], BF16) for i in range(2)]
        gm = [T(f"gm{i}", [128, 8], F32) for i in range(3)]
        m8 = [T(f"m8{i}", [128, 8], F32) for i in range(3)]
        nmpad = [T(f"nmpad{i}", [128, 72], BF16) for i in range(3)]
        nmtmp = [T(f"nmtmp{i}", [128, 8], F32) for i in range(3)]
        colmask = T("colmask", [128, 4, 8], F32)
        gall = [T(f"gall{i}", [128, 64], F32) for i in range(2)]
        psZ = [g.ps[0], g.ps[1]]
        psO = [g.ps[2], g.ps[3]]
        psD = [g.ps[4], g.ps[5]]
        psP = [g.ps[6], g.ps[7]]

        def b(rec):
            rotZ, rotO, rotP, rotPb, rotG = Rot(2), Rot(2), Rot(2), Rot(NB), Rot(3)
            _emit_qkv_weights(g, rec, 1, 0, wq, wk, wv)
            rec.dma("sp", lambda e: e.dma_start(out=b31[:], in_=g.b31), w=["b31"])
            rec.dve(lambda e: e.tensor_scalar(out=nb31[:], in0=b31[:], scalar1=-1.0, scalar2=None, op0=ALU.mult),
                    r=["b31"], w=["nb31"])
            for h in range(NH):
                def one(h=h):
                    bs = bstage[h % 2]
                    rec.dma("sp", lambda e: e.dma_start(out=bs[:], in_=g.biasT[h]), w=[("bstage", h % 2)])
                    rec.act(lambda e: e.activation(out=R[:, h, :, :], in_=bs[:], func=AF.Exp, bias=nb31[:, h:h + 1]),
                            r=[("bstage", h % 2), "nb31"], w=[("R", h)])
                one()
            for i in range(2):
                def one(i=i):
                    rec.pool(lambda e: e.memset(ka[i][64:128, :], 0.0), w=[("ka_oh", i)])
                    rec.pool(lambda e: e.memset(ka[i][64:72, :], 1.0), r=[("ka_oh", i)], w=[("ka_oh", i)])
                    rec.pool(lambda e: e.affine_select(out=ka[i][64:72, :], in_=ka[i][64:72, :], pattern=[[1, S]],
                                                       compare_op=ALU.is_ge, fill=0.0, base=0, channel_multiplier=-256),
                             r=[("ka_oh", i)], w=[("ka_oh", i)])
                    rec.pool(lambda e: e.affine_select(out=ka[i][64:72, :], in_=ka[i][64:72, :], pattern=[[-1, S]],
                                                       compare_op=ALU.is_ge, fill=0.0, base=255, channel_multiplier=256),
                             r=[("ka_oh", i)], w=[("ka_oh", i)])
                    rec.pool(lambda e: e.memset(qa[i][64:128, :], 0.0), w=[("qa_m0", i)])
                one()
            for i in range(3):
                def one(i=i):
                    rec.pool(lambda e: e.memset(nmpad[i][:, 0:64], 0.0), w=[("nmpad0", i)])
                one()
            rec.pool(lambda e: e.memset(colmask[:], 0.0), w=["colmask"])
            for o in range(4, 8):
                def cm(o=o):
                    rec.pool(lambda e: e.memset(colmask[:, o - 4, 0:o], 1.0), r=["colmask"], w=["colmask"])
                cm()

            def proj_head_ops(hp, hh):
                s = hp % 2
                v_ops, k_ops, q_ops = [], [], {tt: [] for tt in range(NTT)}
                if hh == 0:
                    for t4 in range(4):
                        st = {}
                        for j in range(4):
                            for ch in range(2):
                                def vm(t4=t4, j=j, ch=ch, st=st):
                                    if j == 0 and ch == 0:
                                        st["pi"] = rotP.next()
                                    pi = st["pi"]
                                    bank = psP[pi]
                                    tok = t4 * 4 + j
                                    for c in range(ch * 4, ch * 4 + 4):
                                        rec.pe(lambda e, c=c: e.matmul(bank[:, j * 128:(j + 1) * 128],
                                                                       lhsT=g.hnT[:, c, tok * 128:(tok + 1) * 128], rhs=wv[s][:, c, :],
                                                                       start=(c == 0), stop=(c == DC - 1)),
                                               r=[("wv", s), ("hnT", c, tok // 4)], w=[("psP", pi)])
                                    if j == 3 and ch == 1:
                                        rec.dve(lambda e: e.tensor_copy(out=V[s][:, t4 * 4:(t4 + 1) * 4, :],
                                                                        in_=bank[:].rearrange("p (j n) -> p j n", j=4)),
                                                r=[("psP", pi)], w=[("V", s, t4)])
                                v_ops.append(vm)
                for tt in range(NTT):
                    for kind in ("q", "k"):
                        st = {}
                        for c in range(DC):
                            def mm(c=c, tt=tt, kind=kind, st=st):
                                if c == 0:
                                    st["pi"] = rotP.next()
                                pi = st["pi"]
                                bank = psP[pi]
                                wt, wn, dst, dn = (wq, "wq", qa, "qa") if kind == "q" else (wk, "wk", ka, "ka")
                                cs = slice(tt * TT, (tt + 1) * TT)
                                rec.pe(lambda e: e.matmul(bank[0:64, :], lhsT=wt[s][:, c, hh * 64:(hh + 1) * 64],
                                                          rhs=g.hnT[:, c, cs], start=(c == 0), stop=(c == DC - 1)),
                                       r=[(wn, s), ("hnT", c, tt)], w=[("psP", pi)])
                                if c == DC - 1:
                                    if kind == "q":
                                        rec.dve(lambda e: e.tensor_scalar(out=dst[hh][0:64, cs], in0=bank[0:64, :], scalar1=DH ** -0.5,
                                                                          scalar2=None, op0=ALU.mult),
                                                r=[("psP", pi)], w=[(dn, hh, tt)])
                                    else:
                                        rec.dve(lambda e: e.tensor_copy(out=dst[hh][0:64, cs], in_=bank[0:64, :]),
                                                r=[("psP", pi)], w=[(dn, hh, tt)])
                            (q_ops[tt] if kind == "q" else k_ops).append(mm)
                junk = lambda: _emit_fill(g, rec, psP, rotP, 1)
                spacers = [q_ops[0], q_ops[1]] + [v_ops[i * 8:(i + 1) * 8] for i in range(len(v_ops) // 8)]
                ops = k_ops + q_ops[2] + q_ops[3]
                for item in gate_ops(hp, hh):
                    if item is None:
                        ops += spacers.pop(0) if spacers else [junk] * GATE_JUNK
                    else:
                        ops.append(item)
                for grp in spacers:
                    ops += grp
                return ops

            def gate_ops(hp, hh):
                st = {}

                def a1():
                    rec.dve(lambda e: e.tensor_reduce(out=ksum[hh][:], in_=ka[hh][0:64, :].rearrange("p (n k) -> p n k", k=256),
                                                      axis=mybir.AxisListType.X, op=ALU.add),
                            r=[("ka", hh, tt) for tt in range(NTT)], w=[("ksum", hh)])
                    rec.dve(lambda e: e.tensor_copy(out=kmT[hh][:], in_=ksum[hh][:]), r=[("ksum", hh)], w=[("kmT", hh)])

                def a2():
                    pi = rotP.next()
                    gbank = psP[pi]
                    for qt in range(8, 16):
                        def gmm(qt=qt):
                            rec.pe(lambda e: e.matmul(gbank[:, (qt - 8) * 8:(qt - 7) * 8], lhsT=qa[hh][0:64, qt * 128:(qt + 1) * 128],
                                                      rhs=kmT[hh][:], start=True, stop=True),
                                   r=[("qa", hh, qt // 4), ("kmT", hh)], w=[("psP", pi)])
                        gmm()
                    rec.dve(lambda e: e.tensor_copy(out=gall[hh][:], in_=gbank[:, 0:64]), r=[("psP", pi)], w=[("gall", hh)])

                def mk_b(qt):
                    def b_():
                        own = qt // 2
                        gi = rotG.next()
                        st[qt] = gi
                        rec.dve(lambda e: e.tensor_copy(out=gm[gi][:], in_=gall[hh][:, (qt - 8) * 8:(qt - 7) * 8]),
                                r=[("gall", hh)], w=[("gm", gi)])
                        rec.dve(lambda e: e.memset(gm[gi][:, own:8], -1e30), r=[("gm", gi)], w=[("gm", gi)])
                        rec.dve(lambda e: e.max(out=m8[gi][:], in_=gm[gi][:]), r=[("gm", gi)], w=[("m8", gi)])
                        rec.dve(lambda e: e.tensor_scalar(out=nmtmp[gi][:], in0=gm[gi][:], scalar1=m8[gi][:, 2:3],
                                                          scalar2=NEG, op0=ALU.is_lt, op1=ALU.mult),
                                r=[("gm", gi), ("m8", gi)], w=[("nmtmp", gi)])
                        rec.dve(lambda e: e.tensor_tensor(out=nmpad[gi][:, 64:72], in0=nmtmp[gi][:], in1=colmask[:, own - 4, :],
                                                          op=ALU.mult),
                                r=[("nmtmp", gi), "colmask"], w=[("nmpad", gi)])
                    return b_

                def mk_c(qt):
                    def c_():
                        gi = st[qt]
                        mi = rotP.next()
                        mbank = psP[mi]
                        rec.pe(lambda e: e.matmul(mbank[0:72, 0:128], lhsT=nmpad[gi][:, 0:72], rhs=g.ident_b[:],
                                                  start=True, stop=True),
                               r=[("nmpad", gi), ("nmpad0", gi), "consts"], w=[("psP", mi)])
                        rec.act(lambda e: e.activation(out=qa[hh][64:72, qt * 128:(qt + 1) * 128], in_=mbank[64:72, 0:128],
                                                       func=AF.Copy),
                                r=[("psP", mi), ("qa_m0", hh)], w=[("qa_m", hh, qt)])
                    return c_

                seq = [a1, None, a2, None, mk_b(8), mk_b(9), None, mk_c(8)]
                for qt in range(10, 16):
                    seq += [mk_b(qt), None, mk_c(qt - 1)]
                seq += [None, mk_c(15)]
                return seq

            def make_tiles(hp, hh):
                tiles = []
                for tt in range(NTT):
                    chain = []
                    for i, kb in enumerate(range(0, 4 * tt + 4)):
                        j = kb - 4 * tt
                        t = Ctx()
                        t.hp, t.hh, t.tt, t.kb, t.i = hp, hh, tt, kb, i
                        t.c0 = 128 * j if j >= 0 else 0
                        t.last = kb == 4 * tt + 3
                        chain.append(t)
                    tiles += chain
                return tiles

            ostate = {}

            def s1(t):
                c0 = t.c0
                hh, tt, kb = t.hh, t.tt, t.kb
                h = t.hp * 2 + hh
                zb = rotZ.next()
                deps = [("ka", hh, kb // 4), ("ka_oh", hh), ("qa", hh, tt), ("qa_m0", hh)]
                deps += [("qa_m", hh, qt) for qt in range(4 * tt, 4 * tt + 4) if qt >= 8]
                rec.pe(lambda e: e.matmul(psZ[zb][:, c0:], lhsT=ka[hh][:, kb * 128:(kb + 1) * 128],
                                          rhs=qa[hh][:, tt * TT + c0:(tt + 1) * TT], start=True, stop=True),
                       r=deps, w=[("Z", zb)])
                pb = rotPb.next()
                t.pb = pb
                rec.act(lambda e: e.activation(out=P[pb][:, c0:], in_=psZ[zb][:, c0:], func=AF.Exp, bias=b31[:, h:h + 1]),
                        r=[("Z", zb), "b31"], w=[("P", pb)])
                for qi in range(4):
                    delta = 4 * tt + qi - kb
                    if delta in (0, 1) and 128 * qi >= c0:
                        def fix(qi=qi, delta=delta):
                            cols = slice(128 * qi, 128 * (qi + 1))
                            rec.dve(lambda e: e.tensor_tensor(out=P[pb][:, cols], in0=P[pb][:, cols], in1=R[:, h, delta, :],
                                                              op=ALU.mult),
                                    r=[("P", pb), ("R", h)], w=[("P", pb)])
                        fix()

            def s2(t):
                c0 = t.c0
                hh, tt, kb, hp = t.hh, t.tt, t.kb, t.hp
                s = hp % 2
                hb = hh * 64
                M = 128
                first = t.i == 0
                if first:
                    assert c0 == 0
                    ob = rotO.next()
                    ostate["ob"] = ob
                ob = ostate["ob"]
                pb = t.pb
                last = t.last
                rec.pe(lambda e: e.matmul(psO[ob][0:M, c0:], lhsT=V[s][:, kb, 0:M], rhs=P[pb][:, c0:], start=first, stop=last),
                       r=[("V", s, kb // 4), ("P", pb)], w=[("O", ob)])
                rec.pe(lambda e: e.matmul(psD[ob][0:M, c0:], lhsT=g.ones_b[:, 0:M], rhs=P[pb][:, c0:], start=first, stop=last),
                       r=[("P", pb), "consts"], w=[("D", ob)])
                if last:
                    li = ob
                    rec.act(lambda e: e.activation(out=lnd[li][hb:hb + 64, :], in_=psD[ob][hb:hb + 64, :], func=AF.Ln),
                            r=[("D", ob)], w=[("lnd", li)])
                    rec.act(lambda e: e.activation(out=rden[li][hb:hb + 64, :], in_=lnd[li][hb:hb + 64, :], func=AF.Exp, scale=-1.0),
                            r=[("lnd", li)], w=[("rden", li)])
                    rec.dve(lambda e: e.tensor_tensor(out=g.oT[hb:hb + 64, hp, tt * TT:(tt + 1) * TT], in0=psO[ob][hb:hb + 64, :],
                                                      in1=rden[li][hb:hb + 64, :], op=ALU.mult),
                            r=[("O", ob), ("rden", li)], w=[("oT", hp, tt, hh)])

            for op in proj_head_ops(0, 0):
                op()
            for hp in range(DC):
                if hp + 1 < DC:
                    _emit_qkv_weights(g, rec, 1, hp + 1, wq, wk, wv)
                elif fuse_wo:
                    _emit_wo_load(g, rec, wo, 1)
                for hh in range(2):
                    pending = []
                    if hh == 0:
                        pending = proj_head_ops(hp, 1)
                    elif hp + 1 < DC:
                        pending = proj_head_ops(hp + 1, 0)
                    tiles = make_tiles(hp, hh)
                    n = len(tiles)
                    npull = max(FILL_MOBA, -(-len(pending) // (n - 2)))
                    for i in range(n + 2):
                        if i < n:
                            s1(tiles[i])
                        if i >= 2:
                            s2(tiles[i - 2])
                        for k_ in range(npull):
                            if pending:
                                pending.pop(0)()
                            elif k_ < FILL_MOBA:
                                _emit_fill(g, rec, psP, rotP, 1)
                    while pending:
                        pending.pop(0)()
            if fuse_wo:
                _emit_wo(g, rec, wo, psP, rotP)
        phase(g, b)


_NC_CACHE = {}


def kernel(x, w_qkv, w_o, mixer_norm, ffn_norm, rel_bias, w1, w3, w2, router, e_w1, e_w3, e_w2, final_norm):
    inp = dict(x=np.asarray(x), w_qkv=np.asarray(w_qkv), w_o=np.asarray(w_o), mixer_norm=np.asarray(mixer_norm),
               ffn_norm=np.asarray(ffn_norm), rel_bias=np.asarray(rel_bias), w1=np.asarray(w1), w3=np.asarray(w3),
               w2=np.asarray(w2), router=np.asarray(router), e_w1=np.asarray(e_w1), e_w3=np.asarray(e_w3),
               e_w2=np.asarray(e_w2), final_norm=np.asarray(final_norm))
    n = inp["x"].shape[0]
    if "nc" not in _NC_CACHE:
        _NC_CACHE["nc"] = build_nc()
    nc = _NC_CACHE["nc"]
    in_maps = [make_in_map(inp, b) for b in range(n)]
    res = run_bass_kernel_spmd(nc, in_maps, core_ids=list(range(n)))
    return np.stack([np.asarray(r["y"]) for r in res.results], axis=0).astype(np.float32)
```
